# Optimizing a Trainium2 kernel written in Bass

```python
import jax, jax.numpy as jnp
from jax import lax
import numpy as np

D_MODEL = 2048
BATCH = 1
SEQ = 16384
DEPTH = 1

HEAD_DIM = 128
ROT_DIM = HEAD_DIM // 4
ROPE_THETA = 500000.0
DIL_PATTERNS = ((128, 1), (512, 4), (2048, 16))
N_DIL_GROUPS = len(DIL_PATTERNS)
HEADS_PER_GROUP = 4
ATTN_WIDTH = N_DIL_GROUPS * HEADS_PER_GROUP * HEAD_DIM
ATTN_OUT = HEADS_PER_GROUP * HEAD_DIM
ATTN_BLOCK = 128
NEG_INF = -1e30
CHUNK = 128
SGU_GROUPS = 8
SGU_GROUP_DIM = 128
SGU_WIDTH = SGU_GROUPS * SGU_GROUP_DIM
IN_WIDTH = 3 * ATTN_WIDTH + 2 * SGU_WIDTH + 2 * D_MODEL
IN_SPLITS = (ATTN_WIDTH, 2 * ATTN_WIDTH, 3 * ATTN_WIDTH, 3 * ATTN_WIDTH + 2 * SGU_WIDTH,
             3 * ATTN_WIDTH + 2 * SGU_WIDTH + D_MODEL)
N_EXPERTS = 32
TOP_K = 4
D_EXPERT = D_MODEL
SWIGLU_LIMIT = 7.0
SWIGLU_ALPHA = 1.702
MOE_BLOCK = 128
LN_EPS = 1e-5
DEEPNORM_ALPHA = (2 * DEPTH) ** 0.25
DEEPNORM_BETA = (8 * DEPTH) ** -0.25

kernel_name = 'hybrid_dilated_sgu_moe_deepnorm_encoder'


def layer_norm(x, g, b):
    xf = x.astype(jnp.float32)
    mu = jnp.mean(xf, -1, keepdims=True)
    var = jnp.mean(jnp.square(xf - mu), -1, keepdims=True)
    return ((xf - mu) * lax.rsqrt(var + LN_EPS) * g + b).astype(x.dtype)


def rope_partial(t, cos, sin):
    half = ROT_DIM // 2
    r1, r2, rest = t[..., :half], t[..., half:ROT_DIM], t[..., ROT_DIM:]
    rot = jnp.concatenate([r1 * cos - r2 * sin, r2 * cos + r1 * sin], -1).astype(t.dtype)
    return jnp.concatenate([rot, rest], -1)


def banded_attention(q, k, v, half):
    N, L, H, hd = q.shape
    nb = -(-L // ATTN_BLOCK)
    Lp = nb * ATTN_BLOCK
    band = ATTN_BLOCK + 2 * half
    qb = jnp.pad(q, ((0, 0), (0, Lp - L), (0, 0), (0, 0))).reshape(N, nb, ATTN_BLOCK, H, hd)
    pad_kv = ((0, 0), (half, Lp - L + half), (0, 0), (0, 0))
    kp, vp = jnp.pad(k, pad_kv), jnp.pad(v, pad_kv)
    idx = (jnp.arange(nb) * ATTN_BLOCK)[:, None] + jnp.arange(band)[None, :]
    kb, vb = kp[:, idx], vp[:, idx]
    s = jnp.einsum('nbqhd,nbkhd->nbhqk', qb, kb).astype(jnp.float32) * (hd ** -0.5)
    qpos = (jnp.arange(nb) * ATTN_BLOCK)[:, None] + jnp.arange(ATTN_BLOCK)[None, :]
    kpos = idx - half
    rel = kpos[:, None, :] - qpos[:, :, None]
    mask = (jnp.abs(rel) <= half) & (kpos[:, None, :] >= 0) & (kpos[:, None, :] < L)
    s = jnp.where(mask[None, :, None], s, NEG_INF)
    m = jnp.max(s, -1, keepdims=True)
    p = jnp.exp(s - m)
    den = jnp.sum(p, -1, keepdims=True)
    lse = (m + jnp.log(den))[..., 0]
    o = jnp.einsum('nbhqk,nbkhd->nbqhd', (p / den).astype(v.dtype), vb)
    o = o.reshape(N, Lp, H, hd)[:, :L]
    lse = lse.transpose(0, 1, 3, 2).reshape(N, Lp, H)[:, :L]
    return o, lse


def dilated_window_attention(q, k, v, window, dilation):
    B, S, H, hd = q.shape
    L = S // dilation
    def split(t):
        return t.reshape(B, L, dilation, H, hd).transpose(0, 2, 1, 3, 4).reshape(B * dilation, L, H, hd)
    o, lse = banded_attention(split(q), split(k), split(v), window // (2 * dilation))
    o = o.reshape(B, dilation, L, H, hd).transpose(0, 2, 1, 3, 4).reshape(B, S, H, hd)
    lse = lse.reshape(B, dilation, L, H).transpose(0, 2, 1, 3).reshape(B, S, H)
    return o, lse


def token_mixers(x, positions, w_in, w_attn_out, sgu_ln_g, sgu_ln_b, sgu_w, sgu_b, w_sgu_out, w_out):
    B, S, _ = x.shape
    proj = x @ w_in
    q, k, v, z, g_attn, g_sgu = jnp.split(proj, IN_SPLITS, axis=-1)
    inv_freq = ROPE_THETA ** (-jnp.arange(0, ROT_DIM, 2, dtype=jnp.float32) / ROT_DIM)
    ang = positions.astype(jnp.float32)[..., None] * inv_freq
    cos = jnp.cos(ang)[:, :, None, None, :]
    sin = jnp.sin(ang)[:, :, None, None, :]
    shp = (B, S, N_DIL_GROUPS, HEADS_PER_GROUP, HEAD_DIM)
    q = rope_partial(q.reshape(shp), cos, sin)
    k = rope_partial(k.reshape(shp), cos, sin)
    v = v.reshape(shp)
    outs, lses = [], []
    for gi, (window, dilation) in enumerate(DIL_PATTERNS):
        o_g, lse_g = dilated_window_attention(q[:, :, gi], k[:, :, gi], v[:, :, gi], window, dilation)
        outs.append(o_g)
        lses.append(lse_g)
    wts = jax.nn.softmax(jnp.stack(lses, 0), axis=0)
    o_a = jnp.sum(wts[..., None].astype(v.dtype) * jnp.stack(outs, 0), axis=0)
    y_a = o_a.reshape(B, S, ATTN_OUT) @ w_attn_out
    z = jax.nn.gelu(z, approximate=False)
    u, vv = z[..., :SGU_WIDTH], z[..., SGU_WIDTH:]
    vv = layer_norm(vv, sgu_ln_g, sgu_ln_b)
    vc = vv.reshape(B, S // CHUNK, CHUNK, SGU_GROUPS, SGU_GROUP_DIM)
    mixed = jnp.einsum('gts,bnsgc->bntgc', sgu_w, vc) + sgu_b.T[:, :, None]
    y_b = (u * mixed.reshape(B, S, SGU_WIDTH)) @ w_sgu_out
    merged = jax.nn.sigmoid(g_attn) * y_a + jax.nn.sigmoid(g_sgu) * y_b
    return merged @ w_out


def moe(x2d, w_router, b_router, w_gate_up, b_gate_up, w_down, b_down):
    T, D = x2d.shape
    logits = (x2d @ w_router + b_router).astype(jnp.float32)
    top_val, top_idx = lax.top_k(logits, TOP_K)
    gate = jax.nn.softmax(top_val, axis=-1)
    n = T * TOP_K
    flat_e = top_idx.reshape(-1)
    flat_t = jnp.repeat(jnp.arange(T, dtype=jnp.int32), TOP_K)
    flat_w = gate.reshape(-1)
    order = jnp.argsort(flat_e)
    se, st, sw = flat_e[order], flat_t[order], flat_w[order]
    counts = jnp.bincount(flat_e, length=N_EXPERTS)
    padded = (counts + MOE_BLOCK - 1) // MOE_BLOCK * MOE_BLOCK
    starts = jnp.cumsum(counts) - counts
    pends = jnp.cumsum(padded)
    pstarts = pends - padded
    dest = pstarts[se] + (jnp.arange(n) - starts[se])
    n_blocks = -(-n // MOE_BLOCK) + N_EXPERTS
    rows = n_blocks * MOE_BLOCK
    row_tok = jnp.full((rows,), T, jnp.int32).at[dest].set(st)
    row_w = jnp.zeros((rows,), jnp.float32).at[dest].set(sw)
    block_e = jnp.clip(jnp.searchsorted(pends, jnp.arange(n_blocks) * MOE_BLOCK, side='right'),
                       0, N_EXPERTS - 1)
    x_pad = jnp.concatenate([x2d, jnp.zeros((1, D), x2d.dtype)], 0)
    xb = x_pad[row_tok].reshape(n_blocks, MOE_BLOCK, D)

    def expert_block(args):
        xblk, e = args
        gu = xblk @ w_gate_up[e] + b_gate_up[e]
        g, up = gu[:, :D_EXPERT], gu[:, D_EXPERT:]
        g = jnp.minimum(g, SWIGLU_LIMIT)
        up = jnp.clip(up, -SWIGLU_LIMIT, SWIGLU_LIMIT)
        h = (up + 1.0) * (g * jax.nn.sigmoid(SWIGLU_ALPHA * g))
        return h @ w_down[e] + b_down[e]

    yb = lax.map(expert_block, (xb, block_e)).reshape(rows, D)
    y = jax.ops.segment_sum(yb.astype(jnp.float32) * row_w[:, None], row_tok, num_segments=T + 1)[:T]
    return y.astype(x2d.dtype)


def setup_inputs(seed: int = 0) -> dict:
    key = jax.random.key(seed)
    ks = jax.random.split(key, 24)
    f32 = jnp.float32
    def nrm(k, shape, scale):
        return jax.random.normal(k, shape, f32) * scale
    L = DEPTH
    return {
        'x': nrm(ks[0], (BATCH, SEQ, D_MODEL), 1.0),
        'positions': jnp.broadcast_to(jnp.arange(SEQ, dtype=jnp.int32)[None, :], (BATCH, SEQ)),
        'w_in': nrm(ks[1], (L, D_MODEL, IN_WIDTH), D_MODEL ** -0.5),
        'w_attn_out': nrm(ks[2], (L, ATTN_OUT, D_MODEL), ATTN_OUT ** -0.5),
        'sgu_ln_g': 1.0 + nrm(ks[3], (L, SGU_WIDTH), 0.02),
        'sgu_ln_b': nrm(ks[4], (L, SGU_WIDTH), 0.02),
        'sgu_w': nrm(ks[5], (L, SGU_GROUPS, CHUNK, CHUNK), CHUNK ** -0.5),
        'sgu_b': 1.0 + nrm(ks[6], (L, SGU_GROUPS, CHUNK), 0.02),
        'w_sgu_out': nrm(ks[7], (L, SGU_WIDTH, D_MODEL), SGU_WIDTH ** -0.5),
        'w_out': nrm(ks[8], (L, D_MODEL, D_MODEL), DEEPNORM_BETA * D_MODEL ** -0.5),
        'ln1_g': 1.0 + nrm(ks[9], (L, D_MODEL), 0.02),
        'ln1_b': nrm(ks[10], (L, D_MODEL), 0.02),
        'w_router': nrm(ks[11], (L, D_MODEL, N_EXPERTS), D_MODEL ** -0.5),
        'b_router': nrm(ks[12], (L, N_EXPERTS), 0.01),
        'w_gate_up': nrm(ks[13], (L, N_EXPERTS, D_MODEL, 2 * D_EXPERT), D_MODEL ** -0.5),
        'b_gate_up': nrm(ks[14], (L, N_EXPERTS, 2 * D_EXPERT), 0.02),
        'w_down': nrm(ks[15], (L, N_EXPERTS, D_EXPERT, D_MODEL), DEEPNORM_BETA * D_EXPERT ** -0.5),
        'b_down': nrm(ks[16], (L, N_EXPERTS, D_MODEL), 0.02),
        'ln2_g': 1.0 + nrm(ks[17], (L, D_MODEL), 0.02),
        'ln2_b': nrm(ks[18], (L, D_MODEL), 0.02),
    }


def reference(x, positions, w_in, w_attn_out, sgu_ln_g, sgu_ln_b, sgu_w, sgu_b, w_sgu_out, w_out,
              ln1_g, ln1_b, w_router, b_router, w_gate_up, b_gate_up, w_down, b_down, ln2_g, ln2_b):
    B, S, D = x.shape
    for l in range(DEPTH):
        h = token_mixers(x, positions, w_in[l], w_attn_out[l], sgu_ln_g[l], sgu_ln_b[l], sgu_w[l],
                         sgu_b[l], w_sgu_out[l], w_out[l])
        x = layer_norm(DEEPNORM_ALPHA * x + h, ln1_g[l], ln1_b[l])
        y = moe(x.reshape(B * S, D), w_router[l], b_router[l], w_gate_up[l], b_gate_up[l],
                w_down[l], b_down[l]).reshape(B, S, D)
        x = layer_norm(DEEPNORM_ALPHA * x + y, ln2_g[l], ln2_b[l])
    return x
```

```python
import contextlib
import numpy as np
import ml_dtypes
import concourse.bass as bass
import concourse.mybir as mybir
from concourse.bass_utils import run_bass_kernel_spmd

F32, BF16, I32, U32 = mybir.dt.float32, mybir.dt.bfloat16, mybir.dt.int32, mybir.dt.uint32
AF = mybir.ActivationFunctionType
ALU = mybir.AluOpType
AX = mybir.AxisListType

NCORES = 8
D = 2048
SEQ = 16384
TOK = SEQ // NCORES
HALO = 1024
EXT = TOK + 2 * HALO
TT = 512
KC = D // 128
INW = 10752
NE = 32
NEL = NE // NCORES
CAP = 384
NSL = CAP // 128
DH = 2048
DIL = (1, 4, 16)
ALPHA = 2 ** 0.25
EPS = 1e-5
NEG = -30000.0
QOFF, KOFF, VOFF, ZOFF, GAOFF, GBOFF = 0, 1536, 3072, 4608, 6656, 8704


class Ctr:
    def __init__(self, nc, es, name, step):
        self.sem = es.enter_context(nc.semaphore(name))
        self.n = 0
        self.step = step
        self.inorder = False


class Stream:
    def __init__(self, eng, ctr):
        self.eng = eng
        self.ctr = ctr
        self.seen = {}


class Buf:
    def __init__(self, t):
        self.t = t
        self.w = None
        self.r = {}

    def __getitem__(self, k):
        return self.t[k]


class KB:
    def __init__(self, nc, es):
        self.nc, self.es = nc, es
        self.c_pe = Ctr(nc, es, "c_pe", 1)
        self.c_pe.inorder = True
        self.c_act = Ctr(nc, es, "c_act", 1)
        self.c_dve = Ctr(nc, es, "c_dve", 1)
        self.c_pool = Ctr(nc, es, "c_pool", 1)
        self.c_dl = Ctr(nc, es, "c_dl", 16)
        self.c_ds = Ctr(nc, es, "c_ds", 16)
        self.c_dg = Ctr(nc, es, "c_dg", 16)
        self.c_cc = Ctr(nc, es, "c_cc", 1)
        self.pe = Stream(nc.tensor, self.c_pe)
        self.act = Stream(nc.scalar, self.c_act)
        self.dve = Stream(nc.vector, self.c_dve)
        self.pool = Stream(nc.gpsimd, self.c_pool)
        self.sp = Stream(nc.sync, None)
        self.streams = [self.pe, self.act, self.dve, self.pool, self.sp]
        self.ctrs = [self.c_pe, self.c_act, self.c_dve, self.c_pool, self.c_dl, self.c_ds, self.c_dg, self.c_cc]
        self.nbuf = 0

    def sb(self, shape, dt, name=None):
        self.nbuf += 1
        return Buf(self.es.enter_context(self.nc.sbuf_tensor(f"{name or 'sb'}_{self.nbuf}", list(shape), dt)))

    def ps(self, shape, dt, name=None):
        self.nbuf += 1
        return Buf(self.es.enter_context(self.nc.psum_tensor(name or f"ps{self.nbuf}", list(shape), dt)))

    def dram(self, name, shape, dt):
        return Buf(self.nc.dram_tensor(name, list(shape), dt))

    def op(self, st, fn, reads=(), writes=(), ctr=None):
        ctr = ctr or st.ctr
        deps = {}

        def add(d):
            if d is not None:
                c, n = d
                if deps.get(c, 0) < n:
                    deps[c] = n
        for b in reads:
            add(b.w)
        for b in writes:
            add(b.w)
            for c, n in b.r.items():
                add((c, n))
        for c, n in deps.items():
            if c is st.ctr and c.inorder:
                continue
            if st.seen.get(c, 0) >= n:
                continue
            st.eng.wait_ge(c.sem, n)
            st.seen[c] = n
        inst = fn()
        ctr.n += ctr.step
        inst.then_inc(ctr.sem, ctr.step)
        for b in reads:
            if b.r.get(ctr, 0) < ctr.n:
                b.r[ctr] = ctr.n
        for b in writes:
            b.w = (ctr, ctr.n)
            b.r = {}
        return inst

    def barrier(self):
        for st in self.streams:
            for c in self.ctrs:
                if c.n > 0 and st.seen.get(c, 0) < c.n:
                    st.eng.wait_ge(c.sem, c.n)
                    st.seen[c] = c.n

    def mm(self, out, o_ap, lhsT, l_ap, rhs, r_ap, start, stop, extra_reads=()):
        return self.op(self.pe, lambda: self.nc.tensor.matmul(o_ap, lhsT=l_ap, rhs=r_ap, start=start, stop=stop),
                       reads=[lhsT, rhs, *extra_reads], writes=[out])

    def tr(self, out, o_ap, in_, i_ap, ident, id_ap):
        return self.op(self.pe, lambda: self.nc.tensor.transpose(o_ap, i_ap, id_ap), reads=[in_, ident], writes=[out])

    def load(self, out, o_ap, src, s_ap, **kw):
        return self.op(self.sp, lambda: self.nc.sync.dma_start(out=o_ap, in_=s_ap, **kw), reads=[src], writes=[out], ctr=self.c_dl)

    def store(self, out, o_ap, src, s_ap, **kw):
        return self.op(self.sp, lambda: self.nc.sync.dma_start(out=o_ap, in_=s_ap, **kw), reads=[src], writes=[out], ctr=self.c_ds)

    def gdma(self, out, o_ap, src, s_ap, **kw):
        return self.op(self.pool, lambda: self.nc.gpsimd.dma_start(out=o_ap, in_=s_ap, **kw), reads=[src], writes=[out], ctr=self.c_dg)

    def V(self, fn, reads, writes):
        return self.op(self.dve, fn, reads, writes)

    def A(self, fn, reads, writes):
        return self.op(self.act, fn, reads, writes)


def ss(off, step, n=128):
    return slice(off, off + (n - 1) * step + 1, step)


def bcast_rows(ap2d_tensor, row, c0, n, parts=128):
    t = ap2d_tensor
    ncols = t.shape[-1]
    return bass.AP(t, row * ncols + c0, [[0, parts], [1, n]])


def build_nc(stage=99, dbg=False, nvc=NCORES):
    nc = bass.Bass("TRN2", target_bir_lowering=False)

    def din(name, shape, dt=F32):
        return Buf(nc.dram_tensor(name, list(shape), dt, kind="ExternalInput"))

    x_ext = din("x_ext", [nvc * TOK + 2 * HALO, D])
    pos = din("pos", [1, nvc * TOK + 2 * HALO], I32)
    vbias = din("vbias", [nvc, 128, 69])
    w_in = din("w_in", [D, INW])
    w_ao = din("w_ao", [512, D])
    w_so = din("w_so", [1024, D])
    w_o = din("w_o", [D, D])
    if stage >= 4:
        w_gu = din("w_gu", [NE * D, 2 * DH])
        w_d = din("w_d", [NE * DH, D])
    sgu_g = din("sgu_g", [1, 1024])
    sgu_bb = din("sgu_bb", [1, 1024])
    sgu_wT = din("sgu_wT", [128, 8, 128])
    sgu_bias = din("sgu_bias", [1, 1024])
    ln1g = din("ln1g", [1, D]); ln1b = din("ln1b", [1, D])
    ln2g = din("ln2g", [1, D]); ln2b = din("ln2b", [1, D])
    w_r = din("w_r", [128, KC, NE])
    b_r = din("b_r", [1, NE])
    b_gu = din("b_gu", [128, NE, 32])
    b_d = din("b_d", [NE, D])
    c_identb = din("c_identb", [128, 128], BF16)
    c_identf = din("c_identf", [128, 128])
    c_negmask = din("c_negmask", [128, 2, 128], BF16)
    c_rsw = din("c_rsw", [128, 32], BF16)
    c_invf = din("c_invf", [32, 2])
    c_tri = din("c_tri", [128, 128])
    c_onesf = din("c_onesf", [128, 128])
    c_onesb = din("c_onesb", [128, 128], BF16)
    c_iota = din("c_iota", [128, 2, NE])

    out = Buf(nc.dram_tensor("out", [nvc * TOK, D], F32, kind="ExternalOutput"))
    if dbg:
        dbg_oa = Buf(nc.dram_tensor("dbg_oa", [4, 128, TOK], F32, kind="ExternalOutput"))
        dbg_x1 = Buf(nc.dram_tensor("dbg_x1", [TOK, D], F32, kind="ExternalOutput"))

    with contextlib.ExitStack() as es:
        k = KB(nc, es)
        win_g = k.dram("win_g", [D, INW], BF16)
        wao_g = k.dram("wao_g", [512, D], BF16)
        wso_g = k.dram("wso_g", [1024, D], BF16)
        wo_g = k.dram("wo_g", [D, D], BF16)
        xT_d = k.dram("xT_d", [KC, 128, EXT], BF16)
        cs_d = k.dram("cs_d", [2, 32, EXT], F32)
        mg_d = k.dram("mg_d", [KC, 128, TOK], BF16)
        ug_d = k.dram("ug_d", [8, 128, TOK], BF16)
        x1_d = k.dram("x1_d", [TOK, D], F32)
        x1b_d = k.dram("x1b_d", [TOK, D], BF16)
        xg_d = k.dram("xg_d", [NE * CAP, D], BF16)
        yg_d = k.dram("yg_d", [NE * CAP, D], F32)

        def cast(dst, src, rows, cols):
            c2 = max(c for c in range(1, 2049) if cols % c == 0)
            for r0 in range(0, rows, 2048):
                r1 = min(rows, r0 + 2048)
                k.gdma(dst, dst.t.ap()[r0:r1].rearrange("r (a c) -> r a c", c=c2),
                       src, src.t.ap()[r0:r1].rearrange("r (a c) -> r a c", c=c2))

        cast(win_g, w_in, D, INW)
        cast(wao_g, w_ao, 512, D)
        cast(wso_g, w_so, 1024, D)
        cast(wo_g, w_o, D, D)
        wsT = k.sb([128, 8, 128], BF16, "wsT")
        sb_row = k.sb([1, 1024], BF16, "sb_row")
        k.gdma(wsT, wsT[:], sgu_wT, sgu_wT.t.ap())
        k.gdma(sb_row, sb_row[:], sgu_bias, sgu_bias.t.ap())

        identb = k.sb([128, 128], BF16, "identb"); identf = k.sb([128, 128], F32, "identf")
        onesb = k.sb([128, 128], BF16, "onesb"); onesf = k.sb([128, 128], F32, "onesf")
        k.load(identb, identb[:], c_identb, c_identb.t.ap())
        k.load(identf, identf[:], c_identf, c_identf.t.ap())
        k.load(onesb, onesb[:], c_onesb, c_onesb.t.ap())
        k.load(onesf, onesf[:], c_onesf, c_onesf.t.ap())
        psA = k.ps([128, 512], F32, "psA"); psB = k.ps([128, 512], F32, "psB")
        psC = k.ps([128, 512], F32, "psC"); psD = k.ps([128, 512], F32, "psD")
        psTb = k.ps([128, 1024], BF16, "psTb"); psTf = k.ps([128, 512], F32, "psTf")
        psS = k.ps([128, 512], F32, "psS"); psE = k.ps([128, 512], F32, "psE")

        rt_gate = k.sb([128, 16, 4], F32, "rt_gate")
        rt_slot = k.sb([128, 16, 4], I32, "rt_slot")

        def body(vc):
            es_oa = contextlib.ExitStack()
            k.es = es_oa
            oaT = k.sb([128, 4, TOK], BF16, "oaT")
            k.es = es
            if stage < 1:
                return
            with contextlib.ExitStack() as es1:
                k.es = es1
                xin = k.sb([128, D], F32, "xin")
                xtb = k.sb([128, KC, 128], BF16, "xtb")
                for tb in range(EXT // 128):
                    k.load(xin, xin[:], x_ext, x_ext.t.ap()[vc * TOK + tb * 128:vc * TOK + (tb + 1) * 128, :])
                    for kc4 in range(KC // 4):
                        for q in range(4):
                            kc = kc4 * 4 + q
                            k.tr(psTf, psTf[:, q * 128:(q + 1) * 128], xin, xin[:, kc * 128:(kc + 1) * 128], identf, identf[:])
                        k.V(lambda kc4=kc4: nc.vector.tensor_copy(out=xtb[:, kc4 * 4:(kc4 + 1) * 4, :],
                                                                   in_=psTf[:].rearrange("p (a b) -> p a b", b=128)),
                            reads=[psTf], writes=[xtb])
                    k.store(xT_d, xT_d.t.ap()[:, :, tb * 128:(tb + 1) * 128].rearrange("kc p t -> p kc t"), xtb, xtb[:])
                posi = k.sb([32, EXT], I32, "posi"); ang = k.sb([32, EXT], F32, "ang")
                t1 = k.sb([32, EXT], F32, "t1"); ni = k.sb([32, EXT], I32, "ni"); nf = k.sb([32, EXT], F32, "nf")
                invf = k.sb([32, 2], F32, "invf")
                k.load(invf, invf[:], c_invf, c_invf.t.ap())
                k.load(posi, posi[:], pos, bass.AP(pos.t, vc * TOK, [[0, 32], [1, EXT]]))
                k.V(lambda: nc.vector.tensor_copy(out=ang[:], in_=posi[:]), [posi], [ang])
                k.V(lambda: nc.vector.tensor_scalar(out=ang[:], in0=ang[:], scalar1=invf[:, 0:1], scalar2=None, op0=ALU.mult), [ang, invf], [ang])
                C1 = 6.28125
                C2 = float(2 * np.pi - C1)
                for which in range(2):
                    shift = float(np.pi / 2) if which == 0 else 0.0
                    k.V(lambda: nc.vector.tensor_scalar(out=t1[:], in0=ang[:], scalar1=shift, scalar2=float(1 / (2 * np.pi)), op0=ALU.add, op1=ALU.mult), [ang], [t1])
                    k.V(lambda: nc.vector.tensor_copy(out=ni[:], in_=t1[:]), [t1], [ni])
                    k.V(lambda: nc.vector.tensor_copy(out=nf[:], in_=ni[:]), [ni], [nf])
                    k.V(lambda: nc.vector.tensor_scalar(out=t1[:], in0=ang[:], scalar1=shift, scalar2=None, op0=ALU.add), [ang], [t1])
                    k.V(lambda: nc.vector.scalar_tensor_tensor(out=t1[:], in0=nf[:], scalar=-C1, in1=t1[:], op0=ALU.mult, op1=ALU.add), [nf, t1], [t1])
                    k.V(lambda: nc.vector.scalar_tensor_tensor(out=t1[:], in0=nf[:], scalar=-C2, in1=t1[:], op0=ALU.mult, op1=ALU.add), [nf, t1], [t1])
                    k.V(lambda: nc.vector.tensor_scalar(out=nf[:], in0=t1[:], scalar1=float(np.pi), scalar2=float(-2 * np.pi), op0=ALU.is_gt, op1=ALU.mult), [t1], [nf])
                    k.V(lambda: nc.vector.tensor_tensor(out=t1[:], in0=t1[:], in1=nf[:], op=ALU.add), [t1, nf], [t1])
                    k.V(lambda: nc.vector.tensor_scalar(out=nf[:], in0=t1[:], scalar1=float(-np.pi), scalar2=float(2 * np.pi), op0=ALU.is_lt, op1=ALU.mult), [t1], [nf])
                    k.V(lambda: nc.vector.tensor_tensor(out=t1[:], in0=t1[:], in1=nf[:], op=ALU.add), [t1, nf], [t1])
                    k.V(lambda: nc.vector.tensor_scalar(out=t1[:], in0=t1[:], scalar1=float(np.pi), scalar2=float(-np.pi), op0=ALU.min, op1=ALU.max), [t1], [t1])
                    k.A(lambda: nc.scalar.activation(out=nf[:], in_=t1[:], func=AF.Sin), [t1], [nf])
                    if which == 1:
                        k.V(lambda: nc.vector.tensor_scalar(out=nf[:], in0=nf[:], scalar1=invf[:, 1:2], scalar2=None, op0=ALU.mult), [nf, invf], [nf])
                    k.store(cs_d, cs_d.t.ap()[which], nf, nf[:])
                k.barrier()
            k.es = es

            if stage < 2:
                return
            with contextlib.ExitStack() as es2:
                k.es = es2
                negmask = k.sb([128, 2, 128], BF16, "negmask"); rsw = k.sb([128, 32], BF16, "rsw")
                vb = k.sb([128, 69], F32, "vb")
                k.load(negmask, negmask[:], c_negmask, c_negmask.t.ap())
                k.load(rsw, rsw[:], c_rsw, c_rsw.t.ap())
                k.load(vb, vb[:], vbias, vbias.t.ap()[vc])
                wp = k.sb([128, KC, 384], BF16, "wp")
                xt = k.sb([128, KC, TT], BF16, "xt")
                cst = k.sb([32, 2, TT], F32, "cst")
                bufA = k.sb([128, 3, EXT], BF16, "bufA")
                vt = k.sb([128, 69, 128], BF16, "vt")
                qT = k.sb([128, 3, TOK], BF16, "qT")
                acc = k.sb([128, 2, TOK], F32, "acc")
                pT = k.sb([128, 2, 128], BF16, "pT")
                r1 = k.sb([32, TT], F32, "r1"); r2 = k.sb([32, TT], F32, "r2")
                rden = k.sb([128, TOK], F32, "rden")
                kt_idx = {}
                for g, Dl in enumerate(DIL):
                    nb = TOK // Dl // 128
                    for r in range(Dl):
                        for m in range(nb + 1):
                            kt_idx[(g, r, m)] = len(kt_idx)
                assert len(kt_idx) == 69

                def proj(dst, width, col0, tiles, rope):
                    pass

                for h in range(4):
                    for kind in ("v", "k", "q"):
                        base = {"q": QOFF, "k": KOFF, "v": VOFF}[kind]
                        for g in range(3):
                            c0 = base + g * 512 + h * 128
                            k.load(wp, wp[:, :, g * 128:(g + 1) * 128], win_g,
                                   win_g.t.ap()[:, c0:c0 + 128].rearrange("(kc p) c -> p kc c", p=128))
                        tiles = range(2, 6) if kind == "q" else range(0, 8)
                        dst = qT if kind == "q" else bufA
                        for ti in tiles:
                            t0 = ti * TT
                            k.load(xt, xt[:], xT_d, xT_d.t.ap()[:, :, t0:t0 + TT].rearrange("kc p t -> p kc t"))
                            if kind != "v":
                                k.load(cst, cst[:], cs_d, cs_d.t.ap()[:, :, t0:t0 + TT].rearrange("w f t -> f w t"))
                            for g in range(3):
                                if kind != "q" and g < 2 and (ti < 1 or ti > 6):
                                    continue
                                for kc in range(KC):
                                    k.mm(psA, psA[:], wp, wp[:, kc, g * 128:(g + 1) * 128], xt, xt[:, kc, :], kc == 0, kc == KC - 1)
                                d0 = t0 - (HALO if kind == "q" else 0)
                                dap = dst[:, g, d0:d0 + TT]
                                k.A(lambda dap=dap: nc.scalar.activation(out=dap, in_=psA[:], func=AF.Copy), [psA], [dst])
                                if kind != "v":
                                    k.mm(psB, psB[0:32, :], rsw, rsw[:], dst, dap, True, True)
                                    k.V(lambda: nc.vector.tensor_tensor(out=r1[:], in0=psB[0:32, :], in1=cst[:, 1, :], op=ALU.mult), [psB, cst], [r1])
                                    k.V(lambda dap=dap: nc.vector.tensor_tensor(out=r2[:], in0=dst[0:32, g, d0:d0 + TT], in1=cst[:, 0, :], op=ALU.mult), [dst, cst], [r2])
                                    k.V(lambda dap=dap: nc.vector.tensor_tensor(out=dst[0:32, g, d0:d0 + TT], in0=r1[:], in1=r2[:], op=ALU.add), [r1, r2], [dst])
                        if kind == "v":
                            for (g, r, m), ci in kt_idx.items():
                                Dl = DIL[g]
                                off = (HALO // Dl - 64 + 128 * m) * Dl + r
                                src = bufA[:, g, ss(off, Dl)]
                                j = ci % 8
                                k.tr(psTb, psTb[:, j * 128:(j + 1) * 128], bufA, src, identb, identb[:])
                                if j == 7 or ci == 68:
                                    nj = j + 1
                                    c00 = ci - j
                                    k.V(lambda nj=nj, c00=c00: nc.vector.tensor_copy(
                                        out=vt[:, c00:c00 + nj, :], in_=psTb[:, 0:nj * 128].rearrange("p (a b) -> p a b", b=128)),
                                        [psTb], [vt])
                    for g, Dl in enumerate(DIL):
                        nb = TOK // Dl // 128
                        for r in range(Dl):
                            for b in range(nb):
                                qoff = (b * 128) * Dl + r
                                qap = qT[:, g, ss(qoff, Dl)]
                                for half in range(2):
                                    m = b + half
                                    koff = (HALO // Dl - 64 + 128 * m) * Dl + r
                                    kap = bufA[:, g, ss(koff, Dl)]
                                    so = psS[:, half * 128:(half + 1) * 128]
                                    k.mm(psS, so, bufA, kap, qT, qap, True, False)
                                    k.mm(psS, so, identb, identb[:], negmask, negmask[:, half, :], False, True)
                                    ci = kt_idx[(g, r, m)]
                                    k.A(lambda so=so, half=half, ci=ci: nc.scalar.activation(
                                        out=pT[:, half, :], in_=so, func=AF.Exp, bias=vb[:, ci:ci + 1], scale=float(128 ** -0.5)),
                                        [psS, vb], [pT])
                                for half in range(2):
                                    ci = kt_idx[(g, r, b + half)]
                                    k.mm(psC, psC[:, 0:128], vt, vt[:, ci, :], pT, pT[:, half, :], half == 0, half == 1)
                                for half in range(2):
                                    k.mm(psC, psC[:, 128:256], onesb, onesb[:], pT, pT[:, half, :], half == 0, half == 1)
                                aap = acc[:, :, ss(qoff, Dl)]
                                pap = psC[:, 0:256].rearrange("p (a b) -> p a b", b=128)
                                if g == 0:
                                    k.V(lambda aap=aap, pap=pap: nc.vector.tensor_copy(out=aap, in_=pap), [psC], [acc])
                                else:
                                    k.V(lambda aap=aap, pap=pap: nc.vector.tensor_tensor(out=aap, in0=pap, in1=aap, op=ALU.add), [psC, acc], [acc])
                    k.V(lambda: nc.vector.reciprocal(out=rden[:], in_=acc[:, 1, :]), [acc], [rden])
                    k.V(lambda: nc.vector.tensor_tensor(out=acc[:, 0, :], in0=acc[:, 0, :], in1=rden[:], op=ALU.mult), [acc, rden], [acc])
                    k.V(lambda h=h: nc.vector.tensor_copy(out=oaT[:, h, :], in_=acc[:, 0, :]), [acc], [oaT])
                    if dbg and vc == 0:
                        k.store(dbg_oa, dbg_oa.t.ap()[h], acc, acc[:, 0, :])
                k.barrier()
            k.es = es
            if stage < 3:
                return

            with contextlib.ExitStack() as es3:
                k.es = es3
                wz = k.sb([128, KC, 512], BF16, "wz")
                xt = k.sb([128, KC, TT], BF16, "xt3")
                uT = k.sb([128, 8, TT], BF16, "uT")
                vact = k.sb([128, 1024], F32, "vact")
                vall = k.sb([128, 4, 1024], F32, "vall")
                vn = k.sb([128, 4, 1024], BF16, "vn")
                gB = k.sb([128, 1024], F32, "gB"); bB = k.sb([128, 1024], F32, "bB")
                st6 = k.sb([128, 2, 6], F32, "st6"); mv = k.sb([128, 2], F32, "mv"); rstd = k.sb([128, 1], F32, "rstd")
                ugt = k.sb([128, TT], BF16, "ugt")
                k.load(gB, gB[:], sgu_g, bcast_rows(sgu_g.t, 0, 0, 1024))
                k.load(bB, bB[:], sgu_bb, bcast_rows(sgu_bb.t, 0, 0, 1024))
                for ti in range(4):
                    t0 = HALO + ti * TT
                    k.load(xt, xt[:], xT_d, xT_d.t.ap()[:, :, t0:t0 + TT].rearrange("kc p t -> p kc t"))
                    for half in range(2):
                        c0 = ZOFF + half * 512
                        k.load(wz, wz[:], win_g, win_g.t.ap()[:, c0:c0 + 512].rearrange("(kc p) c -> p kc c", p=128))
                        for j in range(4):
                            for kc in range(KC):
                                k.mm(psA, psA[:], wz, wz[:, kc, j * 128:(j + 1) * 128], xt, xt[:, kc, :], kc == 0, kc == KC - 1)
                            k.A(lambda half=half, j=j: nc.scalar.activation(out=uT[:, half * 4 + j, :], in_=psA[:], func=AF.Gelu), [psA], [uT])
                    for half in range(2):
                        c0 = ZOFF + 1024 + half * 512
                        k.load(wz, wz[:], win_g, win_g.t.ap()[:, c0:c0 + 512].rearrange("(kc p) c -> p kc c", p=128))
                        for sblk in range(4):
                            for kc in range(KC):
                                k.mm(psA, psA[:], xt, xt[:, kc, sblk * 128:(sblk + 1) * 128], wz, wz[:, kc, :], kc == 0, kc == KC - 1)
                            k.A(lambda half=half, sblk=sblk: nc.scalar.activation(out=vall[:, sblk, half * 512:(half + 1) * 512], in_=psA[:], func=AF.Gelu), [psA], [vall])
                    for sblk in range(4):
                        for half in range(2):
                            k.V(lambda half=half, sblk=sblk: nc.vector.bn_stats(out=st6[:, half, :], in_=vall[:, sblk, half * 512:(half + 1) * 512]), [vall], [st6])
                        k.V(lambda: nc.vector.bn_aggr(out=mv[:], in_=st6[:].rearrange("p a b -> p (a b)")), [st6], [mv])
                        k.V(lambda: nc.vector.tensor_scalar(out=rstd[:], in0=mv[:, 1:2], scalar1=EPS, scalar2=None, op0=ALU.add), [mv], [rstd])
                        k.A(lambda: nc.scalar.activation(out=rstd[:], in_=rstd[:], func=AF.Sqrt), [rstd], [rstd])
                        k.V(lambda: nc.vector.reciprocal(out=rstd[:], in_=rstd[:]), [rstd], [rstd])
                        k.V(lambda sblk=sblk: nc.vector.tensor_scalar(out=vact[:], in0=vall[:, sblk, :], scalar1=mv[:, 0:1], scalar2=rstd[:, 0:1], op0=ALU.subtract, op1=ALU.mult), [vall, mv, rstd], [vact])
                        k.V(lambda: nc.vector.tensor_tensor(out=vact[:], in0=vact[:], in1=gB[:], op=ALU.mult), [vact, gB], [vact])
                        k.V(lambda sblk=sblk: nc.vector.tensor_tensor(out=vn[:, sblk, :], in0=vact[:], in1=bB[:], op=ALU.add), [vact, bB], [vn])
                    for g in range(8):
                        for sblk in range(4):
                            o = psB[:, sblk * 128:(sblk + 1) * 128]
                            k.mm(psB, o, vn, vn[:, sblk, g * 128:(g + 1) * 128], wsT, wsT[:, g, :], True, False)
                            k.mm(psB, o, onesb, onesb[0:1, :], sb_row, sb_row[0:1, g * 128:(g + 1) * 128], False, True)
                        k.V(lambda g=g: nc.vector.tensor_tensor(out=ugt[:], in0=psB[:], in1=uT[:, g, :], op=ALU.mult), [psB, uT], [ugt])
                        k.store(ug_d, ug_d.t.ap()[g, :, ti * TT:(ti + 1) * TT], ugt, ugt[:])
                k.barrier()
            k.es = es

            with contextlib.ExitStack() as es3:
                k.es = es3
                xto = k.sb([128, KC, TOK], BF16, "xto")
                ugT = k.sb([128, 8, TOK], BF16, "ugT")
                wga = k.sb([128, KC, 128], BF16, "wga"); wgb = k.sb([128, KC, 128], BF16, "wgb")
                wa = k.sb([128, 4, 128], BF16, "wa"); ws = k.sb([128, 8, 128], BF16, "ws")
                sa = k.sb([128, TT], F32, "sa"); sbg = k.sb([128, TT], F32, "sbg")
                m1 = k.sb([128, TT], F32, "m1"); m2 = k.sb([128, TT], F32, "m2"); mgt = k.sb([128, TT], BF16, "mgt")
                k.load(xto, xto[:], xT_d, xT_d.t.ap()[:, :, HALO:HALO + TOK].rearrange("kc p t -> p kc t"))
                k.load(ugT, ugT[:], ug_d, ug_d.t.ap().rearrange("g p t -> p g t"))
                for j in range(KC):
                    k.load(wga, wga[:], win_g, win_g.t.ap()[:, GAOFF + j * 128:GAOFF + (j + 1) * 128].rearrange("(kc p) c -> p kc c", p=128))
                    k.load(wgb, wgb[:], win_g, win_g.t.ap()[:, GBOFF + j * 128:GBOFF + (j + 1) * 128].rearrange("(kc p) c -> p kc c", p=128))
                    k.load(wa, wa[:], wao_g, wao_g.t.ap()[:, j * 128:(j + 1) * 128].rearrange("(kc p) c -> p kc c", p=128))
                    k.load(ws, ws[:], wso_g, wso_g.t.ap()[:, j * 128:(j + 1) * 128].rearrange("(kc p) c -> p kc c", p=128))
                    for ti in range(4):
                        ts = slice(ti * TT, (ti + 1) * TT)
                        for kc in range(KC):
                            k.mm(psA, psA[:], wga, wga[:, kc, :], xto, xto[:, kc, ts], kc == 0, kc == KC - 1)
                        k.A(lambda: nc.scalar.activation(out=sa[:], in_=psA[:], func=AF.Sigmoid), [psA], [sa])
                        for kc in range(KC):
                            k.mm(psB, psB[:], wgb, wgb[:, kc, :], xto, xto[:, kc, ts], kc == 0, kc == KC - 1)
                        k.A(lambda: nc.scalar.activation(out=sbg[:], in_=psB[:], func=AF.Sigmoid), [psB], [sbg])
                        for kc in range(4):
                            k.mm(psC, psC[:], wa, wa[:, kc, :], oaT, oaT[:, kc, ts], kc == 0, kc == 3)
                        for kc in range(8):
                            k.mm(psD, psD[:], ws, ws[:, kc, :], ugT, ugT[:, kc, ts], kc == 0, kc == 7)
                        k.V(lambda: nc.vector.tensor_tensor(out=m1[:], in0=psC[:], in1=sa[:], op=ALU.mult), [psC, sa], [m1])
                        k.V(lambda: nc.vector.tensor_tensor(out=m2[:], in0=psD[:], in1=sbg[:], op=ALU.mult), [psD, sbg], [m2])
                        k.V(lambda: nc.vector.tensor_tensor(out=mgt[:], in0=m1[:], in1=m2[:], op=ALU.add), [m1, m2], [mgt])
                        k.store(mg_d, mg_d.t.ap()[j, :, ts], mgt, mgt[:])
                k.barrier()
            k.es = es

            es_oa.close()
            with contextlib.ExitStack() as es3:
                k.es = es3
                wo = k.sb([128, KC, D], BF16, "wo")
                mt = k.sb([128, KC, 128], BF16, "mt")
                xr = k.sb([128, D], F32, "xr")
                hs = k.sb([128, D], F32, "hs")
                hb = k.sb([128, D], BF16, "hb")
                g1 = k.sb([128, D], F32, "g1"); b1 = k.sb([128, D], F32, "b1")
                st6 = k.sb([128, 4, 6], F32, "st6c"); mv = k.sb([128, 2], F32, "mvc"); rstd = k.sb([128, 1], F32, "rstdc")
                x1T = k.sb([128, KC, 128], F32, "x1T")
                wr = k.sb([128, KC, NE], F32, "wr"); brB = k.sb([128, NE], F32, "brB")
                lg = k.sb([128, NE], F32, "lg"); m8 = k.sb([128, 8], F32, "m8"); i8 = k.sb([128, 8], U32, "i8")
                i8f = k.sb([128, 8], F32, "i8f")
                msk = k.sb([128, 16, NE], F32, "msk")
                ex = k.sb([128, 8], F32, "ex"); ssum = k.sb([128, 1], F32, "ssum"); nmax = k.sb([128, 1], F32, "nmax")
                tri = k.sb([128, 128], F32, "tri")
                iot = k.sb([128, 2, NE], F32, "iot")
                rk = k.sb([128, NE], F32, "rk"); oh = k.sb([128, NE], F32, "oh"); sl = k.sb([128, 4], F32, "sl")
                k.load(wo, wo[:], wo_g, wo_g.t.ap().rearrange("(kc p) c -> p kc c", p=128))
                k.load(g1, g1[:], ln1g, bcast_rows(ln1g.t, 0, 0, D)); k.load(b1, b1[:], ln1b, bcast_rows(ln1b.t, 0, 0, D))
                k.load(wr, wr[:], w_r, w_r.t.ap()); k.load(brB, brB[:], b_r, bcast_rows(b_r.t, 0, 0, NE))
                k.load(tri, tri[:], c_tri, c_tri.t.ap()); k.load(iot, iot[:], c_iota, c_iota.t.ap())
                for blk in range(16):
                    tsl = slice(blk * 128, (blk + 1) * 128)
                    k.load(mt, mt[:], mg_d, mg_d.t.ap()[:, :, tsl].rearrange("kc p t -> p kc t"))
                    k.load(xr, xr[:], x_ext, x_ext.t.ap()[vc * TOK + HALO + blk * 128:vc * TOK + HALO + (blk + 1) * 128, :])
                    for fb, pst in enumerate((psA, psB, psC, psD)):
                        for kc in range(KC):
                            k.mm(pst, pst[:], mt, mt[:, kc, :], wo, wo[:, kc, fb * 512:(fb + 1) * 512], kc == 0, kc == KC - 1)
                        k.V(lambda fb=fb, pst=pst: nc.vector.scalar_tensor_tensor(
                            out=hs[:, fb * 512:(fb + 1) * 512], in0=xr[:, fb * 512:(fb + 1) * 512], scalar=float(ALPHA), in1=pst[:],
                            op0=ALU.mult, op1=ALU.add), [xr, pst], [hs])
                        k.V(lambda fb=fb: nc.vector.bn_stats(out=st6[:, fb, :], in_=hs[:, fb * 512:(fb + 1) * 512]), [hs], [st6])
                    k.V(lambda: nc.vector.bn_aggr(out=mv[:], in_=st6[:].rearrange("p a b -> p (a b)")), [st6], [mv])
                    k.V(lambda: nc.vector.tensor_scalar(out=rstd[:], in0=mv[:, 1:2], scalar1=EPS, scalar2=None, op0=ALU.add), [mv], [rstd])
                    k.A(lambda: nc.scalar.activation(out=rstd[:], in_=rstd[:], func=AF.Sqrt), [rstd], [rstd])
                    k.V(lambda: nc.vector.reciprocal(out=rstd[:], in_=rstd[:]), [rstd], [rstd])
                    k.V(lambda: nc.vector.tensor_scalar(out=hs[:], in0=hs[:], scalar1=mv[:, 0:1], scalar2=rstd[:, 0:1], op0=ALU.subtract, op1=ALU.mult), [hs, mv, rstd], [hs])
                    k.V(lambda: nc.vector.tensor_tensor(out=hs[:], in0=hs[:], in1=g1[:], op=ALU.mult), [hs, g1], [hs])
                    k.V(lambda: nc.vector.tensor_tensor(out=hs[:], in0=hs[:], in1=b1[:], op=ALU.add), [hs, b1], [hs])
                    k.A(lambda: nc.scalar.activation(out=hb[:], in_=hs[:], func=AF.Copy), [hs], [hb])
                    k.store(x1_d, x1_d.t.ap()[tsl, :], hs, hs[:])
                    k.store(x1b_d, x1b_d.t.ap()[tsl, :], hb, hb[:])
                    if dbg and vc == 0:
                        k.store(dbg_x1, dbg_x1.t.ap()[tsl, :], hs, hs[:])
                    if stage < 4:
                        continue
                    for kc4 in range(4):
                        for q in range(4):
                            kc = kc4 * 4 + q
                            k.tr(psTf, psTf[:, q * 128:(q + 1) * 128], hs, hs[:, kc * 128:(kc + 1) * 128], identf, identf[:])
                        k.V(lambda kc4=kc4: nc.vector.tensor_copy(out=x1T[:, kc4 * 4:(kc4 + 1) * 4, :], in_=psTf[:].rearrange("p (a b) -> p a b", b=128)), [psTf], [x1T])
                    for kc in range(KC):
                        k.mm(psS, psS[:, 0:NE], x1T, x1T[:, kc, :], wr, wr[:, kc, :], kc == 0, kc == KC - 1)
                    k.V(lambda: nc.vector.tensor_tensor(out=lg[:], in0=psS[:, 0:NE], in1=brB[:], op=ALU.add), [psS, brB], [lg])
                    k.V(lambda: nc.vector.max(out=m8[:], in_=lg[:]), [lg], [m8])
                    k.V(lambda: nc.vector.max_index(out=i8[:], in_max=m8[:], in_values=lg[:]), [m8, lg], [i8])
                    k.V(lambda: nc.vector.tensor_copy(out=i8f[:], in_=i8[:]), [i8], [i8f])
                    k.V(lambda blk=blk: nc.vector.tensor_scalar(out=msk[:, blk, :], in0=lg[:], scalar1=m8[:, 3:4], scalar2=None, op0=ALU.is_ge), [lg, m8], [msk])
                    k.V(lambda: nc.vector.tensor_scalar(out=nmax[:], in0=m8[:, 0:1], scalar1=-1.0, scalar2=None, op0=ALU.mult), [m8], [nmax])
                    k.A(lambda: nc.scalar.activation(out=ex[:, 0:4], in_=m8[:, 0:4], func=AF.Exp, bias=nmax[:, 0:1], scale=1.0), [m8, nmax], [ex])
                    k.V(lambda: nc.vector.tensor_reduce(out=ssum[:], in_=ex[:, 0:4], axis=AX.X, op=ALU.add), [ex], [ssum])
                    k.V(lambda: nc.vector.reciprocal(out=ssum[:], in_=ssum[:]), [ssum], [ssum])
                    k.V(lambda blk=blk: nc.vector.tensor_scalar(out=rt_gate[:, blk, :], in0=ex[:, 0:4], scalar1=ssum[:, 0:1], scalar2=None, op0=ALU.mult), [ex, ssum], [rt_gate])
                    k.mm(psE, psE[:, 0:NE], tri, tri[:], msk, msk[:, blk, :], True, blk == 0)
                    for pb in range(blk):
                        k.mm(psE, psE[:, 0:NE], onesf, onesf[:], msk, msk[:, pb, :], False, pb == blk - 1)
                    k.V(lambda: nc.vector.tensor_tensor(out=rk[:], in0=psE[:, 0:NE], in1=iot[:, 1, :], op=ALU.add), [psE, iot], [rk])
                    for j in range(4):
                        k.V(lambda j=j: nc.vector.tensor_scalar(out=oh[:], in0=iot[:, 0, :], scalar1=i8f[:, j:j + 1], scalar2=None, op0=ALU.is_equal), [iot, i8f], [oh])
                        k.V(lambda: nc.vector.tensor_tensor(out=oh[:], in0=oh[:], in1=rk[:], op=ALU.mult), [oh, rk], [oh])
                        k.V(lambda j=j: nc.vector.tensor_reduce(out=sl[:, j:j + 1], in_=oh[:], axis=AX.X, op=ALU.add), [oh], [sl])
                    k.V(lambda blk=blk: nc.vector.tensor_copy(out=rt_slot[:, blk, :], in_=sl[:]), [sl], [rt_slot])
                k.barrier()
            k.es = es
            if stage < 4:
                return

            with contextlib.ExitStack() as es4d:
                k.es = es4d
                xb = k.sb([128, D], BF16, "xb4")
                zt = k.sb([128, D], BF16, "zt")
                k.V(lambda: nc.vector.memset(zt[:], 0.0), [], [zt])
                for i in range(NE * CAP // 128):
                    k.store(xg_d, xg_d.t.ap()[i * 128:(i + 1) * 128, :], zt, zt[:])
                for blk in range(16):
                    k.load(xb, xb[:], x1b_d, x1b_d.t.ap()[blk * 128:(blk + 1) * 128, :])
                    for j in range(4):
                        k.op(k.pool, lambda blk=blk, j=j: nc.gpsimd.indirect_dma_start(
                            out=xg_d.t.ap(), out_offset=bass.IndirectOffsetOnAxis(ap=rt_slot[:, blk, j:j + 1], axis=0),
                            in_=xb[:], in_offset=None), reads=[xb, rt_slot], writes=[xg_d], ctr=k.c_dg)
                k.barrier()
            k.es = es
            with contextlib.ExitStack() as es4:
                k.es = es4
                NW = 4
                stg = [k.sb([128, KC, 512], F32, f"stg{i}") for i in range(2)]
                wbf = [k.sb([128, KC, 512], BF16, f"wbf{i}") for i in range(NW)]
                xgt = k.sb([128, NSL, D], BF16, "xgt")
                xgTs = [k.sb([128, KC, CAP], BF16, f"xgT{i}") for i in range(2)]
                hT = k.sb([128, KC, CAP], BF16, "hT")
                bgu = k.sb([128, NE, 32], F32, "bgu")
                tmp = [[k.sb([128, CAP], F32, f"t{n}{i}") for n in ("gc", "sg", "uc")] for i in range(2)]
                bdBs = [k.sb([128, 512], F32, f"bdB{i}") for i in range(2)]
                ysts = [k.sb([128, 512], F32, f"yst{i}") for i in range(2)]
                k.load(bgu, bgu[:], b_gu, b_gu.t.ap())
                wcnt = [0]

                def wload(src, ap):
                    i = wcnt[0]
                    wcnt[0] += 1
                    sgb = stg[i % 2]
                    w = wbf[i % NW]
                    k.load(sgb, sgb[:], src, ap)
                    if i % 2 == 0:
                        k.A(lambda: nc.scalar.activation(out=w[:], in_=sgb[:], func=AF.Copy), [sgb], [w])
                    else:
                        k.op(k.pool, lambda: nc.gpsimd.tensor_copy(out=w[:], in_=sgb[:]), [sgb], [w])
                    return w

                pgu = [(psA, psB), (psC, psD)]
                pdn = [psS, psE]
                hcn = 0
                dn = 0
                for e in range(NE):
                    xgT = xgTs[e % 2]
                    k.load(xgt, xgt[:], xg_d, xg_d.t.ap()[e * CAP:(e + 1) * CAP, :].rearrange("(s p) c -> p s c", p=128))
                    for s in range(NSL):
                        for kc8 in range(2):
                            for q in range(8):
                                kc = kc8 * 8 + q
                                k.tr(psTb, psTb[:, q * 128:(q + 1) * 128], xgt, xgt[:, s, kc * 128:(kc + 1) * 128], identb, identb[:])
                            k.V(lambda s=s, kc8=kc8, xgT=xgT: nc.vector.tensor_copy(out=xgT[:, kc8 * 8:(kc8 + 1) * 8, s * 128:(s + 1) * 128],
                                                                                     in_=psTb[:].rearrange("p (a b) -> p a b", b=128)), [psTb], [xgT])
                    for J in range(4):
                        wg = wload(w_gu, w_gu.t.ap()[e * D:(e + 1) * D, J * 512:(J + 1) * 512].rearrange("(kc p) c -> p kc c", p=128))
                        wu = wload(w_gu, w_gu.t.ap()[e * D:(e + 1) * D, DH + J * 512:DH + (J + 1) * 512].rearrange("(kc p) c -> p kc c", p=128))
                        for jj in range(4):
                            hc = J * 4 + jj
                            pg, pu = pgu[hcn % 2]
                            gc, sg, uc = tmp[hcn % 2]
                            hcn += 1
                            for kc in range(KC):
                                k.mm(pg, pg[:, 0:CAP], wg, wg[:, kc, jj * 128:(jj + 1) * 128], xgT, xgT[:, kc, :], kc == 0, kc == KC - 1)
                            for kc in range(KC):
                                k.mm(pu, pu[:, 0:CAP], wu, wu[:, kc, jj * 128:(jj + 1) * 128], xgT, xgT[:, kc, :], kc == 0, kc == KC - 1)
                            k.V(lambda hc=hc, e=e, pg=pg, gc=gc: nc.vector.tensor_scalar(out=gc[:], in0=pg[:, 0:CAP], scalar1=bgu[:, e, hc:hc + 1], scalar2=7.0, op0=ALU.add, op1=ALU.min), [pg, bgu], [gc])
                            k.A(lambda gc=gc, sg=sg: nc.scalar.activation(out=sg[:], in_=gc[:], func=AF.Sigmoid, scale=1.702), [gc], [sg])
                            k.V(lambda hc=hc, e=e, pu=pu, uc=uc: nc.vector.tensor_scalar(out=uc[:], in0=pu[:, 0:CAP], scalar1=bgu[:, e, 16 + hc:16 + hc + 1], scalar2=7.0, op0=ALU.add, op1=ALU.min), [pu, bgu], [uc])
                            k.V(lambda uc=uc: nc.vector.tensor_scalar(out=uc[:], in0=uc[:], scalar1=-7.0, scalar2=1.0, op0=ALU.max, op1=ALU.add), [uc], [uc])
                            k.V(lambda gc=gc, sg=sg: nc.vector.tensor_tensor(out=gc[:], in0=gc[:], in1=sg[:], op=ALU.mult), [gc, sg], [gc])
                            k.V(lambda hc=hc, gc=gc, uc=uc: nc.vector.tensor_tensor(out=hT[:, hc, :], in0=gc[:], in1=uc[:], op=ALU.mult), [gc, uc], [hT])
                    for F in range(4):
                        wd = wload(w_d, w_d.t.ap()[e * DH:(e + 1) * DH, F * 512:(F + 1) * 512].rearrange("(kc p) c -> p kc c", p=128))
                        bdB = bdBs[F % 2]
                        k.load(bdB, bdB[:], b_d, bcast_rows(b_d.t, e, F * 512, 512))
                        for s in range(NSL):
                            pd = pdn[dn % 2]
                            yst = ysts[dn % 2]
                            dn += 1
                            for kc in range(KC):
                                k.mm(pd, pd[:], hT, hT[:, kc, s * 128:(s + 1) * 128], wd, wd[:, kc, :], kc == 0, kc == KC - 1)
                            k.V(lambda pd=pd, yst=yst, bdB=bdB: nc.vector.tensor_tensor(out=yst[:], in0=pd[:], in1=bdB[:], op=ALU.add), [pd, bdB], [yst])
                            k.store(yg_d, yg_d.t.ap()[e * CAP + s * 128:e * CAP + (s + 1) * 128, F * 512:(F + 1) * 512], yst, yst[:])
                k.barrier()
            k.es = es

            with contextlib.ExitStack() as es5:
                k.es = es5
                x1 = k.sb([128, D], F32, "x1r")
                yj = k.sb([128, D], F32, "yj")
                g2 = k.sb([128, D], F32, "g2"); b2 = k.sb([128, D], F32, "b2")
                st6 = k.sb([128, 4, 6], F32, "st6e"); mv = k.sb([128, 2], F32, "mve"); rstd = k.sb([128, 1], F32, "rstde")
                k.load(g2, g2[:], ln2g, bcast_rows(ln2g.t, 0, 0, D)); k.load(b2, b2[:], ln2b, bcast_rows(ln2b.t, 0, 0, D))
                for blk in range(16):
                    tsl = slice(blk * 128, (blk + 1) * 128)
                    k.load(x1, x1[:], x1_d, x1_d.t.ap()[tsl, :])
                    k.V(lambda: nc.vector.tensor_scalar(out=x1[:], in0=x1[:], scalar1=float(ALPHA), scalar2=None, op0=ALU.mult), [x1], [x1])
                    for j in range(4):
                        k.op(k.pool, lambda blk=blk, j=j: nc.gpsimd.indirect_dma_start(
                            out=yj[:], out_offset=None, in_=yg_d.t.ap(),
                            in_offset=bass.IndirectOffsetOnAxis(ap=rt_slot[:, blk, j:j + 1], axis=0)),
                            reads=[yg_d, rt_slot], writes=[yj], ctr=k.c_dg)
                        k.V(lambda blk=blk, j=j: nc.vector.scalar_tensor_tensor(out=x1[:], in0=yj[:], scalar=rt_gate[:, blk, j:j + 1], in1=x1[:],
                                                                                op0=ALU.mult, op1=ALU.add), [yj, rt_gate, x1], [x1])
                    for fb in range(4):
                        k.V(lambda fb=fb: nc.vector.bn_stats(out=st6[:, fb, :], in_=x1[:, fb * 512:(fb + 1) * 512]), [x1], [st6])
                    k.V(lambda: nc.vector.bn_aggr(out=mv[:], in_=st6[:].rearrange("p a b -> p (a b)")), [st6], [mv])
                    k.V(lambda: nc.vector.tensor_scalar(out=rstd[:], in0=mv[:, 1:2], scalar1=EPS, scalar2=None, op0=ALU.add), [mv], [rstd])
                    k.A(lambda: nc.scalar.activation(out=rstd[:], in_=rstd[:], func=AF.Sqrt), [rstd], [rstd])
                    k.V(lambda: nc.vector.reciprocal(out=rstd[:], in_=rstd[:]), [rstd], [rstd])
                    k.V(lambda: nc.vector.tensor_scalar(out=x1[:], in0=x1[:], scalar1=mv[:, 0:1], scalar2=rstd[:, 0:1], op0=ALU.subtract, op1=ALU.mult), [x1, mv, rstd], [x1])
                    k.V(lambda: nc.vector.tensor_tensor(out=x1[:], in0=x1[:], in1=g2[:], op=ALU.mult), [x1, g2], [x1])
                    k.V(lambda: nc.vector.tensor_tensor(out=x1[:], in0=x1[:], in1=b2[:], op=ALU.add), [x1, b2], [x1])
                    k.store(out, out.t.ap()[vc * TOK + blk * 128:vc * TOK + (blk + 1) * 128, :], x1, x1[:])
            k.es = es

        for vc in range(nvc):
            body(vc)
        _finish(nc, k, out)
    return nc


def _finish(nc, k, out):
    k.barrier()


def _consts():
    bf = ml_dtypes.bfloat16
    c = {}
    c["c_identb"] = np.eye(128, dtype=np.float32).astype(bf)
    c["c_identf"] = np.eye(128, dtype=np.float32)
    i = np.arange(128)[:, None]; j = np.arange(128)[None, :]
    nm = np.zeros((128, 2, 128), np.float32)
    nm[:, 0, :] = np.where(j <= i, 0.0, NEG)
    nm[:, 1, :] = np.where(i <= j, 0.0, NEG)
    c["c_negmask"] = nm.astype(bf)
    rs = np.zeros((128, 32), np.float32)
    for m in range(16):
        rs[m + 16, m] = 1.0
        rs[m, m + 16] = 1.0
    c["c_rsw"] = rs.astype(bf)
    invf = (np.float32(500000.0) ** (-np.arange(0, 32, 2, dtype=np.float32) / np.float32(32))).astype(np.float32)
    cf = np.zeros((32, 2), np.float32)
    cf[:, 0] = np.concatenate([invf, invf])
    cf[:, 1] = np.concatenate([-np.ones(16), np.ones(16)])
    c["c_invf"] = cf
    c["c_tri"] = (i < j).astype(np.float32)
    c["c_onesf"] = np.ones((128, 128), np.float32)
    c["c_onesb"] = np.ones((128, 128), np.float32).astype(bf)
    io = np.zeros((128, 2, NE), np.float32)
    io[:, 0, :] = np.arange(NE)[None, :]
    io[:, 1, :] = np.arange(NE)[None, :] * CAP
    c["c_iota"] = io
    return c


def _vbias(core):
    s = core * TOK
    vb = np.zeros((128, 69), np.float32)
    ci = 0
    for g, Dl in enumerate(DIL):
        nb = TOK // Dl // 128
        for r in range(Dl):
            for m in range(nb + 1):
                off = (HALO // Dl - 64 + 128 * m) * Dl + r
                xs = off + np.arange(128) * Dl
                glob = s - HALO + xs
                vb[:, ci] = np.where((glob >= 0) & (glob < SEQ), 0.0, NEG)
                ci += 1
    return vb


_NC_CACHE = {}
NCK = 8
NVC = NCORES // NCK


def make_in_map(x, positions, w_in, w_attn_out, sgu_ln_g, sgu_ln_b, sgu_w, sgu_b, w_sgu_out, w_out,
                ln1_g, ln1_b, w_router, b_router, w_gate_up, b_gate_up, w_down, b_down, ln2_g, ln2_b):
    f = np.float32
    x2 = np.asarray(x, f).reshape(SEQ, D)
    xpad = np.zeros((SEQ + 2 * HALO, D), f)
    xpad[HALO:HALO + SEQ] = x2
    ppad = np.zeros((1, SEQ + 2 * HALO), np.int32)
    ppad[0, HALO:HALO + SEQ] = np.asarray(positions, np.int32).reshape(SEQ)
    m = {
        "x_ext": xpad, "pos": ppad, "vbias": np.stack([_vbias(c) for c in range(NCORES)], 0),
        "w_in": np.asarray(w_in, f)[0], "w_ao": np.asarray(w_attn_out, f)[0], "w_so": np.asarray(w_sgu_out, f)[0],
        "w_o": np.asarray(w_out, f)[0],
        "w_gu": np.asarray(w_gate_up, f)[0].reshape(NE * D, 2 * DH), "w_d": np.asarray(w_down, f)[0].reshape(NE * DH, D),
        "sgu_g": np.asarray(sgu_ln_g, f).reshape(1, 1024), "sgu_bb": np.asarray(sgu_ln_b, f).reshape(1, 1024),
        "sgu_wT": np.ascontiguousarray(np.asarray(sgu_w, f)[0].transpose(2, 0, 1)),
        "sgu_bias": np.asarray(sgu_b, f).reshape(1, 1024),
        "ln1g": np.asarray(ln1_g, f).reshape(1, D), "ln1b": np.asarray(ln1_b, f).reshape(1, D),
        "ln2g": np.asarray(ln2_g, f).reshape(1, D), "ln2b": np.asarray(ln2_b, f).reshape(1, D),
        "w_r": np.ascontiguousarray(np.asarray(w_router, f)[0].reshape(KC, 128, NE).transpose(1, 0, 2)),
        "b_r": np.asarray(b_router, f).reshape(1, NE),
        "b_gu": np.ascontiguousarray(np.asarray(b_gate_up, f)[0].reshape(NE, 32, 128).transpose(2, 0, 1)),
        "b_d": np.asarray(b_down, f)[0],
        **_consts(),
    }
    return m


def kernel(**inputs):
    m = make_in_map(**inputs)
    span = NVC * TOK
    maps = []
    for c in range(NCK):
        mc = dict(m)
        mc["x_ext"] = m["x_ext"][c * span:c * span + span + 2 * HALO]
        mc["pos"] = np.ascontiguousarray(m["pos"][:, c * span:c * span + span + 2 * HALO])
        mc["vbias"] = np.ascontiguousarray(m["vbias"][c * NVC:(c + 1) * NVC])
        maps.append(mc)
    if "nc" not in _NC_CACHE:
        _NC_CACHE["nc"] = build_nc(nvc=NVC)
    res = run_bass_kernel_spmd(_NC_CACHE["nc"], maps, core_ids=list(range(NCK)))
    outp = np.concatenate([np.asarray(r["out"], np.float32) for r in res.results], axis=0)
    return outp.reshape(1, SEQ, D)
```

```python
import contextlib
import numpy as np
import ml_dtypes
import concourse.bass as bass
import concourse.mybir as mybir
from concourse.bass_utils import run_bass_kernel_spmd

F32, BF16, I32, U32 = mybir.dt.float32, mybir.dt.bfloat16, mybir.dt.int32, mybir.dt.uint32
AF = mybir.ActivationFunctionType
ALU = mybir.AluOpType
AX = mybir.AxisListType

NCORES = 8
D = 2048
SEQ = 16384
TOK = SEQ // NCORES
HALO = 1024
EXT = TOK + 2 * HALO
TT = 512
KC = D // 128
INW = 10752
NE = 32
NEL = NE // NCORES
CAP = 384
NSL = CAP // 128
DH = 2048
DIL = (1, 4, 16)
ALPHA = 2 ** 0.25
EPS = 1e-5
NEG = -30000.0
QOFF, KOFF, VOFF, ZOFF, GAOFF, GBOFF = 0, 1536, 3072, 4608, 6656, 8704


class Ctr:
    def __init__(self, nc, es, name, step):
        self.sem = es.enter_context(nc.semaphore(name))
        self.n = 0
        self.step = step
        self.inorder = False


class Stream:
    def __init__(self, eng, ctr):
        self.eng = eng
        self.ctr = ctr
        self.seen = {}


class Buf:
    def __init__(self, t):
        self.t = t
        self.w = None
        self.r = {}

    def __getitem__(self, k):
        return self.t[k]


class KB:
    def __init__(self, nc, es):
        self.nc, self.es = nc, es
        self.c_pe = Ctr(nc, es, "c_pe", 1)
        self.c_pe.inorder = True
        self.c_act = Ctr(nc, es, "c_act", 1)
        self.c_dve = Ctr(nc, es, "c_dve", 1)
        self.c_pool = Ctr(nc, es, "c_pool", 1)
        self.c_dl = Ctr(nc, es, "c_dl", 16)
        self.c_ds = Ctr(nc, es, "c_ds", 16)
        self.c_dg = Ctr(nc, es, "c_dg", 16)
        self.c_cc = Ctr(nc, es, "c_cc", 1)
        self.pe = Stream(nc.tensor, self.c_pe)
        self.act = Stream(nc.scalar, self.c_act)
        self.dve = Stream(nc.vector, self.c_dve)
        self.pool = Stream(nc.gpsimd, self.c_pool)
        self.sp = Stream(nc.sync, None)
        self.streams = [self.pe, self.act, self.dve, self.pool, self.sp]
        self.ctrs = [self.c_pe, self.c_act, self.c_dve, self.c_pool, self.c_dl, self.c_ds, self.c_dg, self.c_cc]
        self.nbuf = 0

    def sb(self, shape, dt, name=None):
        self.nbuf += 1
        return Buf(self.es.enter_context(self.nc.sbuf_tensor(f"{name or 'sb'}_{self.nbuf}", list(shape), dt)))

    def ps(self, shape, dt, name=None):
        self.nbuf += 1
        return Buf(self.es.enter_context(self.nc.psum_tensor(name or f"ps{self.nbuf}", list(shape), dt)))

    def dram(self, name, shape, dt):
        return Buf(self.nc.dram_tensor(name, list(shape), dt))

    def op(self, st, fn, reads=(), writes=(), ctr=None):
        ctr = ctr or st.ctr
        deps = {}

        def add(d):
            if d is not None:
                c, n = d
                if deps.get(c, 0) < n:
                    deps[c] = n
        for b in reads:
            add(b.w)
        for b in writes:
            add(b.w)
            for c, n in b.r.items():
                add((c, n))
        for c, n in deps.items():
            if c is st.ctr and c.inorder:
                continue
            if st.seen.get(c, 0) >= n:
                continue
            st.eng.wait_ge(c.sem, n)
            st.seen[c] = n
        inst = fn()
        ctr.n += ctr.step
        inst.then_inc(ctr.sem, ctr.step)
        for b in reads:
            if b.r.get(ctr, 0) < ctr.n:
                b.r[ctr] = ctr.n
        for b in writes:
            b.w = (ctr, ctr.n)
            b.r = {}
        return inst

    def barrier(self):
        for st in self.streams:
            for c in self.ctrs:
                if c.n > 0 and st.seen.get(c, 0) < c.n:
                    st.eng.wait_ge(c.sem, c.n)
                    st.seen[c] = c.n

    def mm(self, out, o_ap, lhsT, l_ap, rhs, r_ap, start, stop, extra_reads=()):
        return self.op(self.pe, lambda: self.nc.tensor.matmul(o_ap, lhsT=l_ap, rhs=r_ap, start=start, stop=stop),
                       reads=[lhsT, rhs, *extra_reads], writes=[out])

    def tr(self, out, o_ap, in_, i_ap, ident, id_ap):
        return self.op(self.pe, lambda: self.nc.tensor.transpose(o_ap, i_ap, id_ap), reads=[in_, ident], writes=[out])

    def load(self, out, o_ap, src, s_ap, **kw):
        return self.op(self.sp, lambda: self.nc.sync.dma_start(out=o_ap, in_=s_ap, **kw), reads=[src], writes=[out], ctr=self.c_dl)

    def store(self, out, o_ap, src, s_ap, **kw):
        return self.op(self.pool, lambda: self.nc.gpsimd.dma_start(out=o_ap, in_=s_ap, **kw), reads=[src], writes=[out], ctr=self.c_ds)

    def gdma(self, out, o_ap, src, s_ap, **kw):
        return self.op(self.pool, lambda: self.nc.gpsimd.dma_start(out=o_ap, in_=s_ap, **kw), reads=[src], writes=[out], ctr=self.c_dg)

    def V(self, fn, reads, writes):
        return self.op(self.dve, fn, reads, writes)

    def A(self, fn, reads, writes):
        return self.op(self.act, fn, reads, writes)


def ss(off, step, n=128):
    return slice(off, off + (n - 1) * step + 1, step)


def bcast_rows(ap2d_tensor, row, c0, n, parts=128):
    t = ap2d_tensor
    ncols = t.shape[-1]
    return bass.AP(t, row * ncols + c0, [[0, parts], [1, n]])


def build_nc(stage=99, dbg=False, nvc=NCORES):
    nc = bass.Bass("TRN2", target_bir_lowering=False)

    def din(name, shape, dt=F32):
        return Buf(nc.dram_tensor(name, list(shape), dt, kind="ExternalInput"))

    x_ext = din("x_ext", [nvc * TOK + 2 * HALO, D])
    pos = din("pos", [1, nvc * TOK + 2 * HALO], I32)
    vbias = din("vbias", [nvc, 128, 69])
    w_in = din("w_in", [D, INW])
    w_ao = din("w_ao", [512, D])
    w_so = din("w_so", [1024, D])
    w_o = din("w_o", [D, D])
    if stage >= 4:
        w_gu = din("w_gu", [NE * D, 2 * DH])
        w_d = din("w_d", [NE * DH, D])
    sgu_g = din("sgu_g", [1, 1024])
    sgu_bb = din("sgu_bb", [1, 1024])
    sgu_wT = din("sgu_wT", [128, 8, 128])
    sgu_bias = din("sgu_bias", [1, 1024])
    ln1g = din("ln1g", [1, D]); ln1b = din("ln1b", [1, D])
    ln2g = din("ln2g", [1, D]); ln2b = din("ln2b", [1, D])
    w_r = din("w_r", [128, KC, NE])
    b_r = din("b_r", [1, NE])
    b_gu = din("b_gu", [128, NE, 32])
    b_d = din("b_d", [NE, D])
    c_identb = din("c_identb", [128, 128], BF16)
    c_identf = din("c_identf", [128, 128])
    c_negmask = din("c_negmask", [128, 2, 128], BF16)
    c_rsw = din("c_rsw", [128, 32], BF16)
    c_invf = din("c_invf", [32, 2])
    c_tri = din("c_tri", [128, 128])
    c_onesf = din("c_onesf", [128, 128])
    c_onesb = din("c_onesb", [128, 128], BF16)
    c_iota = din("c_iota", [128, 2, NE])

    out = Buf(nc.dram_tensor("out", [nvc * TOK, D], F32, kind="ExternalOutput"))
    if dbg:
        dbg_oa = Buf(nc.dram_tensor("dbg_oa", [4, 128, TOK], F32, kind="ExternalOutput"))
        dbg_x1 = Buf(nc.dram_tensor("dbg_x1", [TOK, D], F32, kind="ExternalOutput"))

    with contextlib.ExitStack() as es:
        k = KB(nc, es)
        win_g = k.dram("win_g", [D, INW], BF16)
        wao_g = k.dram("wao_g", [512, D], BF16)
        wso_g = k.dram("wso_g", [1024, D], BF16)
        wo_g = k.dram("wo_g", [D, D], BF16)
        xT_d = k.dram("xT_d", [KC, 128, EXT], BF16)
        cs_d = k.dram("cs_d", [2, 32, EXT], F32)
        mg_d = k.dram("mg_d", [KC, 128, TOK], BF16)
        ug_d = k.dram("ug_d", [8, 128, TOK], BF16)
        x1_d = k.dram("x1_d", [TOK, D], F32)
        x1b_d = k.dram("x1b_d", [TOK, D], BF16)
        xg_d = k.dram("xg_d", [NE * CAP, D], BF16)
        yg_d = k.dram("yg_d", [NE * CAP, D], F32)

        def cast(dst, src, rows, cols):
            c2 = max(c for c in range(1, 2049) if cols % c == 0)
            for r0 in range(0, rows, 2048):
                r1 = min(rows, r0 + 2048)
                k.gdma(dst, dst.t.ap()[r0:r1].rearrange("r (a c) -> r a c", c=c2),
                       src, src.t.ap()[r0:r1].rearrange("r (a c) -> r a c", c=c2))

        cast(win_g, w_in, D, INW)
        cast(wao_g, w_ao, 512, D)
        cast(wso_g, w_so, 1024, D)
        cast(wo_g, w_o, D, D)
        wsT = k.sb([128, 8, 128], BF16, "wsT")
        sb_row = k.sb([1, 1024], BF16, "sb_row")
        k.gdma(wsT, wsT[:], sgu_wT, sgu_wT.t.ap())
        k.gdma(sb_row, sb_row[:], sgu_bias, sgu_bias.t.ap())

        identb = k.sb([128, 128], BF16, "identb"); identf = k.sb([128, 128], F32, "identf")
        onesb = k.sb([128, 128], BF16, "onesb"); onesf = k.sb([128, 128], F32, "onesf")
        k.load(identb, identb[:], c_identb, c_identb.t.ap())
        k.load(identf, identf[:], c_identf, c_identf.t.ap())
        k.load(onesb, onesb[:], c_onesb, c_onesb.t.ap())
        k.load(onesf, onesf[:], c_onesf, c_onesf.t.ap())
        psA = k.ps([128, 512], F32, "psA"); psB = k.ps([128, 512], F32, "psB")
        psC = k.ps([128, 512], F32, "psC"); psD = k.ps([128, 512], F32, "psD")
        psTb = k.ps([128, 1024], BF16, "psTb"); psTf = k.ps([128, 512], F32, "psTf")
        psS = k.ps([128, 512], F32, "psS"); psE = k.ps([128, 512], F32, "psE")

        rt_gate = k.sb([128, 16, 4], F32, "rt_gate")
        rt_slot = k.sb([128, 16, 4], I32, "rt_slot")

        def body(vc):
            es_oa = contextlib.ExitStack()
            k.es = es_oa
            oaT = k.sb([128, 4, TOK], BF16, "oaT")
            k.es = es
            if stage < 1:
                return
            with contextlib.ExitStack() as es1:
                k.es = es1
                xin = k.sb([128, D], F32, "xin")
                xtb = k.sb([128, KC, 128], BF16, "xtb")
                for tb in range(EXT // 128):
                    k.load(xin, xin[:], x_ext, x_ext.t.ap()[vc * TOK + tb * 128:vc * TOK + (tb + 1) * 128, :])
                    for kc4 in range(KC // 4):
                        for q in range(4):
                            kc = kc4 * 4 + q
                            k.tr(psTf, psTf[:, q * 128:(q + 1) * 128], xin, xin[:, kc * 128:(kc + 1) * 128], identf, identf[:])
                        k.V(lambda kc4=kc4: nc.vector.tensor_copy(out=xtb[:, kc4 * 4:(kc4 + 1) * 4, :],
                                                                   in_=psTf[:].rearrange("p (a b) -> p a b", b=128)),
                            reads=[psTf], writes=[xtb])
                    k.store(xT_d, xT_d.t.ap()[:, :, tb * 128:(tb + 1) * 128].rearrange("kc p t -> p kc t"), xtb, xtb[:])
                posi = k.sb([32, EXT], I32, "posi"); ang = k.sb([32, EXT], F32, "ang")
                t1 = k.sb([32, EXT], F32, "t1"); ni = k.sb([32, EXT], I32, "ni"); nf = k.sb([32, EXT], F32, "nf")
                invf = k.sb([32, 2], F32, "invf")
                k.load(invf, invf[:], c_invf, c_invf.t.ap())
                k.load(posi, posi[:], pos, bass.AP(pos.t, vc * TOK, [[0, 32], [1, EXT]]))
                k.V(lambda: nc.vector.tensor_copy(out=ang[:], in_=posi[:]), [posi], [ang])
                k.V(lambda: nc.vector.tensor_scalar(out=ang[:], in0=ang[:], scalar1=invf[:, 0:1], scalar2=None, op0=ALU.mult), [ang, invf], [ang])
                C1 = 6.28125
                C2 = float(2 * np.pi - C1)
                for which in range(2):
                    shift = float(np.pi / 2) if which == 0 else 0.0
                    k.V(lambda: nc.vector.tensor_scalar(out=t1[:], in0=ang[:], scalar1=shift, scalar2=float(1 / (2 * np.pi)), op0=ALU.add, op1=ALU.mult), [ang], [t1])
                    k.V(lambda: nc.vector.tensor_copy(out=ni[:], in_=t1[:]), [t1], [ni])
                    k.V(lambda: nc.vector.tensor_copy(out=nf[:], in_=ni[:]), [ni], [nf])
                    k.V(lambda: nc.vector.tensor_scalar(out=t1[:], in0=ang[:], scalar1=shift, scalar2=None, op0=ALU.add), [ang], [t1])
                    k.V(lambda: nc.vector.scalar_tensor_tensor(out=t1[:], in0=nf[:], scalar=-C1, in1=t1[:], op0=ALU.mult, op1=ALU.add), [nf, t1], [t1])
                    k.V(lambda: nc.vector.scalar_tensor_tensor(out=t1[:], in0=nf[:], scalar=-C2, in1=t1[:], op0=ALU.mult, op1=ALU.add), [nf, t1], [t1])
                    k.V(lambda: nc.vector.tensor_scalar(out=nf[:], in0=t1[:], scalar1=float(np.pi), scalar2=float(-2 * np.pi), op0=ALU.is_gt, op1=ALU.mult), [t1], [nf])
                    k.V(lambda: nc.vector.tensor_tensor(out=t1[:], in0=t1[:], in1=nf[:], op=ALU.add), [t1, nf], [t1])
                    k.V(lambda: nc.vector.tensor_scalar(out=nf[:], in0=t1[:], scalar1=float(-np.pi), scalar2=float(2 * np.pi), op0=ALU.is_lt, op1=ALU.mult), [t1], [nf])
                    k.V(lambda: nc.vector.tensor_tensor(out=t1[:], in0=t1[:], in1=nf[:], op=ALU.add), [t1, nf], [t1])
                    k.V(lambda: nc.vector.tensor_scalar(out=t1[:], in0=t1[:], scalar1=float(np.pi), scalar2=float(-np.pi), op0=ALU.min, op1=ALU.max), [t1], [t1])
                    k.A(lambda: nc.scalar.activation(out=nf[:], in_=t1[:], func=AF.Sin), [t1], [nf])
                    if which == 1:
                        k.V(lambda: nc.vector.tensor_scalar(out=nf[:], in0=nf[:], scalar1=invf[:, 1:2], scalar2=None, op0=ALU.mult), [nf, invf], [nf])
                    k.store(cs_d, cs_d.t.ap()[which], nf, nf[:])
                k.barrier()
            k.es = es

            if stage < 2:
                return
            with contextlib.ExitStack() as es2:
                k.es = es2
                negmask = k.sb([128, 2, 128], BF16, "negmask"); rsw = k.sb([128, 32], BF16, "rsw")
                vb = k.sb([128, 69], F32, "vb")
                k.load(negmask, negmask[:], c_negmask, c_negmask.t.ap())
                k.load(rsw, rsw[:], c_rsw, c_rsw.t.ap())
                k.load(vb, vb[:], vbias, vbias.t.ap()[vc])
                wp = k.sb([128, KC, 384], BF16, "wp")
                xts = [k.sb([128, KC, TT], BF16, f"xt{i}") for i in range(2)]
                csts = [k.sb([32, 2, TT], F32, f"cst{i}") for i in range(2)]
                pproj = [psA, psD]
                rot = {"x": 0, "p": 0, "b": 0}
                bufA = k.sb([128, 3, EXT], BF16, "bufA")
                vt = k.sb([128, 69, 128], BF16, "vt")
                qT = k.sb([128, 3, TOK], BF16, "qT")
                acc = k.sb([128, 2, TOK], F32, "acc")
                pTs = [k.sb([128, 2, 128], BF16, f"pT{i}") for i in range(2)]
                r1 = k.sb([32, TT], F32, "r1"); r2 = k.sb([32, TT], F32, "r2")
                rden = k.sb([128, TOK], F32, "rden")
                kt_idx = {}
                for g, Dl in enumerate(DIL):
                    nb = TOK // Dl // 128
                    for r in range(Dl):
                        for m in range(nb + 1):
                            kt_idx[(g, r, m)] = len(kt_idx)
                assert len(kt_idx) == 69

                def proj(dst, width, col0, tiles, rope):
                    pass

                for h in range(4):
                    for kind in ("v", "k", "q"):
                        base = {"q": QOFF, "k": KOFF, "v": VOFF}[kind]
                        for g in range(3):
                            c0 = base + g * 512 + h * 128
                            k.load(wp, wp[:, :, g * 128:(g + 1) * 128], win_g,
                                   win_g.t.ap()[:, c0:c0 + 128].rearrange("(kc p) c -> p kc c", p=128))
                        tiles = range(2, 6) if kind == "q" else range(0, 8)
                        dst = qT if kind == "q" else bufA

                        def rope_stage(dst, g, d0, cst):
                            dap = dst[:, g, d0:d0 + TT]
                            k.mm(psB, psB[0:32, :], rsw, rsw[:], dst, dap, True, True)
                            k.V(lambda: nc.vector.tensor_tensor(out=r1[:], in0=psB[0:32, :], in1=cst[:, 1, :], op=ALU.mult), [psB, cst], [r1])
                            k.V(lambda: nc.vector.tensor_tensor(out=r2[:], in0=dst[0:32, g, d0:d0 + TT], in1=cst[:, 0, :], op=ALU.mult), [dst, cst], [r2])
                            k.V(lambda: nc.vector.tensor_tensor(out=dst[0:32, g, d0:d0 + TT], in0=r1[:], in1=r2[:], op=ALU.add), [r1, r2], [dst])

                        pending = None
                        for ti in tiles:
                            t0 = ti * TT
                            xt = xts[rot["x"] % 2]
                            cst = csts[rot["x"] % 2]
                            rot["x"] += 1
                            k.load(xt, xt[:], xT_d, xT_d.t.ap()[:, :, t0:t0 + TT].rearrange("kc p t -> p kc t"))
                            if kind != "v":
                                k.load(cst, cst[:], cs_d, cs_d.t.ap()[:, :, t0:t0 + TT].rearrange("w f t -> f w t"))
                            for g in range(3):
                                if kind != "q" and g < 2 and (ti < 1 or ti > 6):
                                    continue
                                pp = pproj[rot["p"] % 2]
                                rot["p"] += 1
                                for kc in range(KC):
                                    k.mm(pp, pp[:], wp, wp[:, kc, g * 128:(g + 1) * 128], xt, xt[:, kc, :], kc == 0, kc == KC - 1)
                                d0 = t0 - (HALO if kind == "q" else 0)
                                dap = dst[:, g, d0:d0 + TT]
                                k.A(lambda dap=dap, pp=pp: nc.scalar.activation(out=dap, in_=pp[:], func=AF.Copy), [pp], [dst])
                                if kind != "v":
                                    if pending is not None:
                                        rope_stage(*pending)
                                    pending = (dst, g, d0, cst)
                        if pending is not None:
                            rope_stage(*pending)
                        if kind == "v":
                            for (g, r, m), ci in kt_idx.items():
                                Dl = DIL[g]
                                off = (HALO // Dl - 64 + 128 * m) * Dl + r
                                src = bufA[:, g, ss(off, Dl)]
                                j = ci % 8
                                k.tr(psTb, psTb[:, j * 128:(j + 1) * 128], bufA, src, identb, identb[:])
                                if j == 7 or ci == 68:
                                    nj = j + 1
                                    c00 = ci - j
                                    k.V(lambda nj=nj, c00=c00: nc.vector.tensor_copy(
                                        out=vt[:, c00:c00 + nj, :], in_=psTb[:, 0:nj * 128].rearrange("p (a b) -> p a b", b=128)),
                                        [psTb], [vt])
                    pss = [psS, psE]
                    pcs = [psC, psTf]

                    def pv_stage(pT, pc, g, r, b, qoff, Dl):
                        for half in range(2):
                            ci = kt_idx[(g, r, b + half)]
                            k.mm(pc, pc[:, 0:128], vt, vt[:, ci, :], pT, pT[:, half, :], half == 0, half == 1)
                        for half in range(2):
                            k.mm(pc, pc[:, 128:256], onesb, onesb[:], pT, pT[:, half, :], half == 0, half == 1)
                        aap = acc[:, :, ss(qoff, Dl)]
                        pap = pc[:, 0:256].rearrange("p (a b) -> p a b", b=128)
                        if g == 0:
                            k.V(lambda: nc.vector.tensor_copy(out=aap, in_=pap), [pc], [acc])
                        else:
                            k.V(lambda: nc.vector.tensor_tensor(out=aap, in0=pap, in1=aap, op=ALU.add), [pc, acc], [acc])

                    pend = None
                    for g, Dl in enumerate(DIL):
                        nb = TOK // Dl // 128
                        for r in range(Dl):
                            for b in range(nb):
                                bi = rot["b"]
                                rot["b"] += 1
                                psc = pss[bi % 2]
                                pT = pTs[bi % 2]
                                qoff = (b * 128) * Dl + r
                                qap = qT[:, g, ss(qoff, Dl)]
                                for half in range(2):
                                    m = b + half
                                    koff = (HALO // Dl - 64 + 128 * m) * Dl + r
                                    kap = bufA[:, g, ss(koff, Dl)]
                                    so = psc[:, half * 128:(half + 1) * 128]
                                    k.mm(psc, so, bufA, kap, qT, qap, True, False)
                                    k.mm(psc, so, identb, identb[:], negmask, negmask[:, half, :], False, True)
                                    ci = kt_idx[(g, r, m)]
                                    k.A(lambda so=so, half=half, ci=ci, pT=pT: nc.scalar.activation(
                                        out=pT[:, half, :], in_=so, func=AF.Exp, bias=vb[:, ci:ci + 1], scale=float(128 ** -0.5)),
                                        [psc, vb], [pT])
                                if pend is not None:
                                    pv_stage(*pend)
                                pend = (pT, pcs[bi % 2], g, r, b, qoff, Dl)
                    if pend is not None:
                        pv_stage(*pend)
                    k.V(lambda: nc.vector.reciprocal(out=rden[:], in_=acc[:, 1, :]), [acc], [rden])
                    k.V(lambda: nc.vector.tensor_tensor(out=acc[:, 0, :], in0=acc[:, 0, :], in1=rden[:], op=ALU.mult), [acc, rden], [acc])
                    k.V(lambda h=h: nc.vector.tensor_copy(out=oaT[:, h, :], in_=acc[:, 0, :]), [acc], [oaT])
                    if dbg and vc == 0:
                        k.store(dbg_oa, dbg_oa.t.ap()[h], acc, acc[:, 0, :])
                k.barrier()
            k.es = es
            if stage < 3:
                return

            with contextlib.ExitStack() as es3:
                k.es = es3
                wzs = [k.sb([128, KC, 512], BF16, f"wz{i}") for i in range(2)]
                wzi = [0]
                xt = k.sb([128, KC, TT], BF16, "xt3")
                uT = k.sb([128, 8, TT], BF16, "uT")
                vact = k.sb([128, 1024], F32, "vact")
                vall = k.sb([128, 4, 1024], F32, "vall")
                vn = k.sb([128, 4, 1024], BF16, "vn")
                gB = k.sb([128, 1024], F32, "gB"); bB = k.sb([128, 1024], F32, "bB")
                st6 = k.sb([128, 2, 6], F32, "st6"); mv = k.sb([128, 2], F32, "mv"); rstd = k.sb([128, 1], F32, "rstd")
                ugt = k.sb([128, TT], BF16, "ugt")
                k.load(gB, gB[:], sgu_g, bcast_rows(sgu_g.t, 0, 0, 1024))
                k.load(bB, bB[:], sgu_bb, bcast_rows(sgu_bb.t, 0, 0, 1024))
                for ti in range(4):
                    t0 = HALO + ti * TT
                    k.load(xt, xt[:], xT_d, xT_d.t.ap()[:, :, t0:t0 + TT].rearrange("kc p t -> p kc t"))
                    for half in range(2):
                        c0 = ZOFF + half * 512
                        wz = wzs[wzi[0] % 2]
                        wzi[0] += 1
                        k.load(wz, wz[:], win_g, win_g.t.ap()[:, c0:c0 + 512].rearrange("(kc p) c -> p kc c", p=128))
                        for j in range(4):
                            for kc in range(KC):
                                k.mm(psA, psA[:], wz, wz[:, kc, j * 128:(j + 1) * 128], xt, xt[:, kc, :], kc == 0, kc == KC - 1)
                            k.A(lambda half=half, j=j: nc.scalar.activation(out=uT[:, half * 4 + j, :], in_=psA[:], func=AF.Gelu), [psA], [uT])
                    for half in range(2):
                        c0 = ZOFF + 1024 + half * 512
                        wz = wzs[wzi[0] % 2]
                        wzi[0] += 1
                        k.load(wz, wz[:], win_g, win_g.t.ap()[:, c0:c0 + 512].rearrange("(kc p) c -> p kc c", p=128))
                        for sblk in range(4):
                            for kc in range(KC):
                                k.mm(psA, psA[:], xt, xt[:, kc, sblk * 128:(sblk + 1) * 128], wz, wz[:, kc, :], kc == 0, kc == KC - 1)
                            k.A(lambda half=half, sblk=sblk: nc.scalar.activation(out=vall[:, sblk, half * 512:(half + 1) * 512], in_=psA[:], func=AF.Gelu), [psA], [vall])
                    for sblk in range(4):
                        for half in range(2):
                            k.V(lambda half=half, sblk=sblk: nc.vector.bn_stats(out=st6[:, half, :], in_=vall[:, sblk, half * 512:(half + 1) * 512]), [vall], [st6])
                        k.V(lambda: nc.vector.bn_aggr(out=mv[:], in_=st6[:].rearrange("p a b -> p (a b)")), [st6], [mv])
                        k.V(lambda: nc.vector.tensor_scalar(out=rstd[:], in0=mv[:, 1:2], scalar1=EPS, scalar2=None, op0=ALU.add), [mv], [rstd])
                        k.A(lambda: nc.scalar.activation(out=rstd[:], in_=rstd[:], func=AF.Sqrt), [rstd], [rstd])
                        k.V(lambda: nc.vector.reciprocal(out=rstd[:], in_=rstd[:]), [rstd], [rstd])
                        k.V(lambda sblk=sblk: nc.vector.tensor_scalar(out=vact[:], in0=vall[:, sblk, :], scalar1=mv[:, 0:1], scalar2=rstd[:, 0:1], op0=ALU.subtract, op1=ALU.mult), [vall, mv, rstd], [vact])
                        k.V(lambda: nc.vector.tensor_tensor(out=vact[:], in0=vact[:], in1=gB[:], op=ALU.mult), [vact, gB], [vact])
                        k.V(lambda sblk=sblk: nc.vector.tensor_tensor(out=vn[:, sblk, :], in0=vact[:], in1=bB[:], op=ALU.add), [vact, bB], [vn])
                    for g in range(8):
                        for sblk in range(4):
                            o = psB[:, sblk * 128:(sblk + 1) * 128]
                            k.mm(psB, o, vn, vn[:, sblk, g * 128:(g + 1) * 128], wsT, wsT[:, g, :], True, False)
                            k.mm(psB, o, onesb, onesb[0:1, :], sb_row, sb_row[0:1, g * 128:(g + 1) * 128], False, True)
                        k.V(lambda g=g: nc.vector.tensor_tensor(out=ugt[:], in0=psB[:], in1=uT[:, g, :], op=ALU.mult), [psB, uT], [ugt])
                        k.store(ug_d, ug_d.t.ap()[g, :, ti * TT:(ti + 1) * TT], ugt, ugt[:])
                k.barrier()
            k.es = es

            with contextlib.ExitStack() as es3:
                k.es = es3
                xto = k.sb([128, KC, TOK], BF16, "xto")
                ugT = k.sb([128, 8, TOK], BF16, "ugT")
                wgas = [k.sb([128, KC, 128], BF16, f"wga{i}") for i in range(2)]; wgbs = [k.sb([128, KC, 128], BF16, f"wgb{i}") for i in range(2)]
                was = [k.sb([128, 4, 128], BF16, f"wa{i}") for i in range(2)]; wss = [k.sb([128, 8, 128], BF16, f"ws{i}") for i in range(2)]
                sa = k.sb([128, TT], F32, "sa"); sbg = k.sb([128, TT], F32, "sbg")
                m1 = k.sb([128, TT], F32, "m1"); m2 = k.sb([128, TT], F32, "m2"); mgt = k.sb([128, TT], BF16, "mgt")
                k.load(xto, xto[:], xT_d, xT_d.t.ap()[:, :, HALO:HALO + TOK].rearrange("kc p t -> p kc t"))
                k.load(ugT, ugT[:], ug_d, ug_d.t.ap().rearrange("g p t -> p g t"))
                for j in range(KC):
                    wga, wgb, wa, ws = wgas[j % 2], wgbs[j % 2], was[j % 2], wss[j % 2]
                    k.load(wga, wga[:], win_g, win_g.t.ap()[:, GAOFF + j * 128:GAOFF + (j + 1) * 128].rearrange("(kc p) c -> p kc c", p=128))
                    k.load(wgb, wgb[:], win_g, win_g.t.ap()[:, GBOFF + j * 128:GBOFF + (j + 1) * 128].rearrange("(kc p) c -> p kc c", p=128))
                    k.load(wa, wa[:], wao_g, wao_g.t.ap()[:, j * 128:(j + 1) * 128].rearrange("(kc p) c -> p kc c", p=128))
                    k.load(ws, ws[:], wso_g, wso_g.t.ap()[:, j * 128:(j + 1) * 128].rearrange("(kc p) c -> p kc c", p=128))
                    for ti in range(4):
                        ts = slice(ti * TT, (ti + 1) * TT)
                        for kc in range(KC):
                            k.mm(psA, psA[:], wga, wga[:, kc, :], xto, xto[:, kc, ts], kc == 0, kc == KC - 1)
                        k.A(lambda: nc.scalar.activation(out=sa[:], in_=psA[:], func=AF.Sigmoid), [psA], [sa])
                        for kc in range(KC):
                            k.mm(psB, psB[:], wgb, wgb[:, kc, :], xto, xto[:, kc, ts], kc == 0, kc == KC - 1)
                        k.A(lambda: nc.scalar.activation(out=sbg[:], in_=psB[:], func=AF.Sigmoid), [psB], [sbg])
                        for kc in range(4):
                            k.mm(psC, psC[:], wa, wa[:, kc, :], oaT, oaT[:, kc, ts], kc == 0, kc == 3)
                        for kc in range(8):
                            k.mm(psD, psD[:], ws, ws[:, kc, :], ugT, ugT[:, kc, ts], kc == 0, kc == 7)
                        k.V(lambda: nc.vector.tensor_tensor(out=m1[:], in0=psC[:], in1=sa[:], op=ALU.mult), [psC, sa], [m1])
                        k.V(lambda: nc.vector.tensor_tensor(out=m2[:], in0=psD[:], in1=sbg[:], op=ALU.mult), [psD, sbg], [m2])
                        k.V(lambda: nc.vector.tensor_tensor(out=mgt[:], in0=m1[:], in1=m2[:], op=ALU.add), [m1, m2], [mgt])
                        k.store(mg_d, mg_d.t.ap()[j, :, ts], mgt, mgt[:])
                k.barrier()
            k.es = es

            es_oa.close()
            with contextlib.ExitStack() as es3:
                k.es = es3
                wo = k.sb([128, KC, D], BF16, "wo")
                mts = [k.sb([128, KC, 128], BF16, f"mt{i}") for i in range(2)]
                xrs = [k.sb([128, D], F32, f"xr{i}") for i in range(2)]
                hs = k.sb([128, D], F32, "hs")
                hb = k.sb([128, D], BF16, "hb")
                g1 = k.sb([128, D], F32, "g1"); b1 = k.sb([128, D], F32, "b1")
                st6 = k.sb([128, 4, 6], F32, "st6c"); mv = k.sb([128, 2], F32, "mvc"); rstd = k.sb([128, 1], F32, "rstdc")
                x1T = k.sb([128, KC, 128], F32, "x1T")
                wr = k.sb([128, KC, NE], F32, "wr"); brB = k.sb([128, NE], F32, "brB")
                lg = k.sb([128, NE], F32, "lg"); m8 = k.sb([128, 8], F32, "m8"); i8 = k.sb([128, 8], U32, "i8")
                i8f = k.sb([128, 8], F32, "i8f")
                msk = k.sb([128, 16, NE], F32, "msk")
                ex = k.sb([128, 8], F32, "ex"); ssum = k.sb([128, 1], F32, "ssum"); nmax = k.sb([128, 1], F32, "nmax")
                tri = k.sb([128, 128], F32, "tri")
                iot = k.sb([128, 2, NE], F32, "iot")
                rk = k.sb([128, NE], F32, "rk"); oh = k.sb([128, NE], F32, "oh"); sl = k.sb([128, 4], F32, "sl")
                k.load(wo, wo[:], wo_g, wo_g.t.ap().rearrange("(kc p) c -> p kc c", p=128))
                k.load(g1, g1[:], ln1g, bcast_rows(ln1g.t, 0, 0, D)); k.load(b1, b1[:], ln1b, bcast_rows(ln1b.t, 0, 0, D))
                k.load(wr, wr[:], w_r, w_r.t.ap()); k.load(brB, brB[:], b_r, bcast_rows(b_r.t, 0, 0, NE))
                k.load(tri, tri[:], c_tri, c_tri.t.ap()); k.load(iot, iot[:], c_iota, c_iota.t.ap())
                for blk in range(16):
                    tsl = slice(blk * 128, (blk + 1) * 128)
                    mt, xr = mts[blk % 2], xrs[blk % 2]
                    k.load(mt, mt[:], mg_d, mg_d.t.ap()[:, :, tsl].rearrange("kc p t -> p kc t"))
                    k.load(xr, xr[:], x_ext, x_ext.t.ap()[vc * TOK + HALO + blk * 128:vc * TOK + HALO + (blk + 1) * 128, :])
                    for fb, pst in enumerate((psA, psB, psC, psD)):
                        for kc in range(KC):
                            k.mm(pst, pst[:], mt, mt[:, kc, :], wo, wo[:, kc, fb * 512:(fb + 1) * 512], kc == 0, kc == KC - 1)
                        k.V(lambda fb=fb, pst=pst: nc.vector.scalar_tensor_tensor(
                            out=hs[:, fb * 512:(fb + 1) * 512], in0=xr[:, fb * 512:(fb + 1) * 512], scalar=float(ALPHA), in1=pst[:],
                            op0=ALU.mult, op1=ALU.add), [xr, pst], [hs])
                        k.V(lambda fb=fb: nc.vector.bn_stats(out=st6[:, fb, :], in_=hs[:, fb * 512:(fb + 1) * 512]), [hs], [st6])
                    k.V(lambda: nc.vector.bn_aggr(out=mv[:], in_=st6[:].rearrange("p a b -> p (a b)")), [st6], [mv])
                    k.V(lambda: nc.vector.tensor_scalar(out=rstd[:], in0=mv[:, 1:2], scalar1=EPS, scalar2=None, op0=ALU.add), [mv], [rstd])
                    k.A(lambda: nc.scalar.activation(out=rstd[:], in_=rstd[:], func=AF.Sqrt), [rstd], [rstd])
                    k.V(lambda: nc.vector.reciprocal(out=rstd[:], in_=rstd[:]), [rstd], [rstd])
                    k.V(lambda: nc.vector.tensor_scalar(out=hs[:], in0=hs[:], scalar1=mv[:, 0:1], scalar2=rstd[:, 0:1], op0=ALU.subtract, op1=ALU.mult), [hs, mv, rstd], [hs])
                    k.V(lambda: nc.vector.tensor_tensor(out=hs[:], in0=hs[:], in1=g1[:], op=ALU.mult), [hs, g1], [hs])
                    k.V(lambda: nc.vector.tensor_tensor(out=hs[:], in0=hs[:], in1=b1[:], op=ALU.add), [hs, b1], [hs])
                    k.A(lambda: nc.scalar.activation(out=hb[:], in_=hs[:], func=AF.Copy), [hs], [hb])
                    k.store(x1_d, x1_d.t.ap()[tsl, :], hs, hs[:])
                    k.store(x1b_d, x1b_d.t.ap()[tsl, :], hb, hb[:])
                    if dbg and vc == 0:
                        k.store(dbg_x1, dbg_x1.t.ap()[tsl, :], hs, hs[:])
                    if stage < 4:
                        continue
                    for kc4 in range(4):
                        for q in range(4):
                            kc = kc4 * 4 + q
                            k.tr(psTf, psTf[:, q * 128:(q + 1) * 128], hs, hs[:, kc * 128:(kc + 1) * 128], identf, identf[:])
                        k.V(lambda kc4=kc4: nc.vector.tensor_copy(out=x1T[:, kc4 * 4:(kc4 + 1) * 4, :], in_=psTf[:].rearrange("p (a b) -> p a b", b=128)), [psTf], [x1T])
                    for kc in range(KC):
                        k.mm(psS, psS[:, 0:NE], x1T, x1T[:, kc, :], wr, wr[:, kc, :], kc == 0, kc == KC - 1)
                    k.V(lambda: nc.vector.tensor_tensor(out=lg[:], in0=psS[:, 0:NE], in1=brB[:], op=ALU.add), [psS, brB], [lg])
                    k.V(lambda: nc.vector.max(out=m8[:], in_=lg[:]), [lg], [m8])
                    k.V(lambda: nc.vector.max_index(out=i8[:], in_max=m8[:], in_values=lg[:]), [m8, lg], [i8])
                    k.V(lambda: nc.vector.tensor_copy(out=i8f[:], in_=i8[:]), [i8], [i8f])
                    k.V(lambda blk=blk: nc.vector.tensor_scalar(out=msk[:, blk, :], in0=lg[:], scalar1=m8[:, 3:4], scalar2=None, op0=ALU.is_ge), [lg, m8], [msk])
                    k.V(lambda: nc.vector.tensor_scalar(out=nmax[:], in0=m8[:, 0:1], scalar1=-1.0, scalar2=None, op0=ALU.mult), [m8], [nmax])
                    k.A(lambda: nc.scalar.activation(out=ex[:, 0:4], in_=m8[:, 0:4], func=AF.Exp, bias=nmax[:, 0:1], scale=1.0), [m8, nmax], [ex])
                    k.V(lambda: nc.vector.tensor_reduce(out=ssum[:], in_=ex[:, 0:4], axis=AX.X, op=ALU.add), [ex], [ssum])
                    k.V(lambda: nc.vector.reciprocal(out=ssum[:], in_=ssum[:]), [ssum], [ssum])
                    k.V(lambda blk=blk: nc.vector.tensor_scalar(out=rt_gate[:, blk, :], in0=ex[:, 0:4], scalar1=ssum[:, 0:1], scalar2=None, op0=ALU.mult), [ex, ssum], [rt_gate])
                    k.mm(psE, psE[:, 0:NE], tri, tri[:], msk, msk[:, blk, :], True, blk == 0)
                    for pb in range(blk):
                        k.mm(psE, psE[:, 0:NE], onesf, onesf[:], msk, msk[:, pb, :], False, pb == blk - 1)
                    k.V(lambda: nc.vector.tensor_tensor(out=rk[:], in0=psE[:, 0:NE], in1=iot[:, 1, :], op=ALU.add), [psE, iot], [rk])
                    for j in range(4):
                        k.V(lambda j=j: nc.vector.tensor_scalar(out=oh[:], in0=iot[:, 0, :], scalar1=i8f[:, j:j + 1], scalar2=None, op0=ALU.is_equal), [iot, i8f], [oh])
                        k.V(lambda: nc.vector.tensor_tensor(out=oh[:], in0=oh[:], in1=rk[:], op=ALU.mult), [oh, rk], [oh])
                        k.V(lambda j=j: nc.vector.tensor_reduce(out=sl[:, j:j + 1], in_=oh[:], axis=AX.X, op=ALU.add), [oh], [sl])
                    k.V(lambda blk=blk: nc.vector.tensor_copy(out=rt_slot[:, blk, :], in_=sl[:]), [sl], [rt_slot])
                k.barrier()
            k.es = es
            if stage < 4:
                return

            with contextlib.ExitStack() as es4d:
                k.es = es4d
                xb = k.sb([128, D], BF16, "xb4")
                zt = k.sb([128, D], BF16, "zt")
                k.V(lambda: nc.vector.memset(zt[:], 0.0), [], [zt])
                for i in range(NE * CAP // 128):
                    k.store(xg_d, xg_d.t.ap()[i * 128:(i + 1) * 128, :], zt, zt[:])
                for blk in range(16):
                    k.load(xb, xb[:], x1b_d, x1b_d.t.ap()[blk * 128:(blk + 1) * 128, :])
                    for j in range(4):
                        k.op(k.pool, lambda blk=blk, j=j: nc.gpsimd.indirect_dma_start(
                            out=xg_d.t.ap(), out_offset=bass.IndirectOffsetOnAxis(ap=rt_slot[:, blk, j:j + 1], axis=0),
                            in_=xb[:], in_offset=None), reads=[xb, rt_slot], writes=[xg_d], ctr=k.c_dg)
                k.barrier()
            k.es = es
            with contextlib.ExitStack() as es4:
                k.es = es4
                NW = 4
                stg = [k.sb([128, KC, 512], F32, f"stg{i}") for i in range(2)]
                wbf = [k.sb([128, KC, 512], BF16, f"wbf{i}") for i in range(NW)]
                xgt = k.sb([128, NSL, D], BF16, "xgt")
                xgTs = [k.sb([128, KC, CAP], BF16, f"xgT{i}") for i in range(2)]
                hT = k.sb([128, KC, CAP], BF16, "hT")
                bgu = k.sb([128, NE, 32], F32, "bgu")
                tmp = [[k.sb([128, CAP], F32, f"t{n}{i}") for n in ("gc", "sg", "uc")] for i in range(2)]
                bdBs = [k.sb([128, 512], F32, f"bdB{i}") for i in range(2)]
                ysts = [k.sb([128, 512], F32, f"yst{i}") for i in range(2)]
                k.load(bgu, bgu[:], b_gu, b_gu.t.ap())
                wcnt = [0]

                def wload(src, ap):
                    i = wcnt[0]
                    wcnt[0] += 1
                    sgb = stg[i % 2]
                    w = wbf[i % NW]
                    k.load(sgb, sgb[:], src, ap)
                    k.A(lambda: nc.scalar.activation(out=w[:], in_=sgb[:], func=AF.Copy), [sgb], [w])
                    return w

                pgu = [(psA, psB), (psC, psD)]
                pdn = [psS, psE]
                hcn = 0
                dn = 0
                blocks = []
                for e in range(NE):
                    for J in range(4):
                        blocks.append((w_gu, w_gu.t.ap()[e * D:(e + 1) * D, J * 512:(J + 1) * 512].rearrange("(kc p) c -> p kc c", p=128)))
                        blocks.append((w_gu, w_gu.t.ap()[e * D:(e + 1) * D, DH + J * 512:DH + (J + 1) * 512].rearrange("(kc p) c -> p kc c", p=128)))
                    for F in range(4):
                        blocks.append((w_d, w_d.t.ap()[e * DH:(e + 1) * DH, F * 512:(F + 1) * 512].rearrange("(kc p) c -> p kc c", p=128)))
                wbl = {}

                def ensure(n):
                    while wcnt[0] < min(n, len(blocks)):
                        i = wcnt[0]
                        wbl[i] = wload(*blocks[i])
                bidx = 0
                for e in range(NE):
                    xgT = xgTs[e % 2]
                    k.load(xgt, xgt[:], xg_d, xg_d.t.ap()[e * CAP:(e + 1) * CAP, :].rearrange("(s p) c -> p s c", p=128))
                    for s in range(NSL):
                        for kc8 in range(2):
                            for q in range(8):
                                kc = kc8 * 8 + q
                                k.tr(psTb, psTb[:, q * 128:(q + 1) * 128], xgt, xgt[:, s, kc * 128:(kc + 1) * 128], identb, identb[:])
                            k.V(lambda s=s, kc8=kc8, xgT=xgT: nc.vector.tensor_copy(out=xgT[:, kc8 * 8:(kc8 + 1) * 8, s * 128:(s + 1) * 128],
                                                                                     in_=psTb[:].rearrange("p (a b) -> p a b", b=128)), [psTb], [xgT])
                    for J in range(4):
                        ensure(bidx + 4)
                        wg = wbl.pop(bidx)
                        wu = wbl.pop(bidx + 1)
                        bidx += 2
                        for jj in range(4):
                            hc = J * 4 + jj
                            pg, pu = pgu[hcn % 2]
                            gc, sg, uc = tmp[hcn % 2]
                            hcn += 1
                            for kc in range(KC):
                                k.mm(pg, pg[:, 0:CAP], wg, wg[:, kc, jj * 128:(jj + 1) * 128], xgT, xgT[:, kc, :], kc == 0, kc == KC - 1)
                            for kc in range(KC):
                                k.mm(pu, pu[:, 0:CAP], wu, wu[:, kc, jj * 128:(jj + 1) * 128], xgT, xgT[:, kc, :], kc == 0, kc == KC - 1)
                            k.V(lambda hc=hc, e=e, pg=pg, gc=gc: nc.vector.tensor_scalar(out=gc[:], in0=pg[:, 0:CAP], scalar1=bgu[:, e, hc:hc + 1], scalar2=7.0, op0=ALU.add, op1=ALU.min), [pg, bgu], [gc])
                            k.A(lambda gc=gc, sg=sg: nc.scalar.activation(out=sg[:], in_=gc[:], func=AF.Sigmoid, scale=1.702), [gc], [sg])
                            k.V(lambda hc=hc, e=e, pu=pu, uc=uc: nc.vector.tensor_scalar(out=uc[:], in0=pu[:, 0:CAP], scalar1=bgu[:, e, 16 + hc:16 + hc + 1], scalar2=7.0, op0=ALU.add, op1=ALU.min), [pu, bgu], [uc])
                            k.V(lambda uc=uc: nc.vector.tensor_scalar(out=uc[:], in0=uc[:], scalar1=-7.0, scalar2=1.0, op0=ALU.max, op1=ALU.add), [uc], [uc])
                            k.V(lambda gc=gc, sg=sg: nc.vector.tensor_tensor(out=gc[:], in0=gc[:], in1=sg[:], op=ALU.mult), [gc, sg], [gc])
                            k.V(lambda hc=hc, gc=gc, uc=uc: nc.vector.tensor_tensor(out=hT[:, hc, :], in0=gc[:], in1=uc[:], op=ALU.mult), [gc, uc], [hT])
                    for F in range(4):
                        ensure(bidx + 3)
                        wd = wbl.pop(bidx)
                        bidx += 1
                        bdB = bdBs[F % 2]
                        k.gdma(bdB, bdB[:], b_d, bcast_rows(b_d.t, e, F * 512, 512))
                        for s in range(NSL):
                            pd = pdn[dn % 2]
                            yst = ysts[dn % 2]
                            dn += 1
                            for kc in range(KC):
                                k.mm(pd, pd[:], hT, hT[:, kc, s * 128:(s + 1) * 128], wd, wd[:, kc, :], kc == 0, kc == KC - 1)
                            k.V(lambda pd=pd, yst=yst, bdB=bdB: nc.vector.tensor_tensor(out=yst[:], in0=pd[:], in1=bdB[:], op=ALU.add), [pd, bdB], [yst])
                            k.store(yg_d, yg_d.t.ap()[e * CAP + s * 128:e * CAP + (s + 1) * 128, F * 512:(F + 1) * 512], yst, yst[:])
                k.barrier()
            k.es = es

            with contextlib.ExitStack() as es5:
                k.es = es5
                x1 = k.sb([128, D], F32, "x1r")
                yj = k.sb([128, D], F32, "yj")
                g2 = k.sb([128, D], F32, "g2"); b2 = k.sb([128, D], F32, "b2")
                st6 = k.sb([128, 4, 6], F32, "st6e"); mv = k.sb([128, 2], F32, "mve"); rstd = k.sb([128, 1], F32, "rstde")
                k.load(g2, g2[:], ln2g, bcast_rows(ln2g.t, 0, 0, D)); k.load(b2, b2[:], ln2b, bcast_rows(ln2b.t, 0, 0, D))
                for blk in range(16):
                    tsl = slice(blk * 128, (blk + 1) * 128)
                    k.load(x1, x1[:], x1_d, x1_d.t.ap()[tsl, :])
                    k.V(lambda: nc.vector.tensor_scalar(out=x1[:], in0=x1[:], scalar1=float(ALPHA), scalar2=None, op0=ALU.mult), [x1], [x1])
                    for j in range(4):
                        k.op(k.pool, lambda blk=blk, j=j: nc.gpsimd.indirect_dma_start(
                            out=yj[:], out_offset=None, in_=yg_d.t.ap(),
                            in_offset=bass.IndirectOffsetOnAxis(ap=rt_slot[:, blk, j:j + 1], axis=0)),
                            reads=[yg_d, rt_slot], writes=[yj], ctr=k.c_dg)
                        k.V(lambda blk=blk, j=j: nc.vector.scalar_tensor_tensor(out=x1[:], in0=yj[:], scalar=rt_gate[:, blk, j:j + 1], in1=x1[:],
                                                                                op0=ALU.mult, op1=ALU.add), [yj, rt_gate, x1], [x1])
                    for fb in range(4):
                        k.V(lambda fb=fb: nc.vector.bn_stats(out=st6[:, fb, :], in_=x1[:, fb * 512:(fb + 1) * 512]), [x1], [st6])
                    k.V(lambda: nc.vector.bn_aggr(out=mv[:], in_=st6[:].rearrange("p a b -> p (a b)")), [st6], [mv])
                    k.V(lambda: nc.vector.tensor_scalar(out=rstd[:], in0=mv[:, 1:2], scalar1=EPS, scalar2=None, op0=ALU.add), [mv], [rstd])
                    k.A(lambda: nc.scalar.activation(out=rstd[:], in_=rstd[:], func=AF.Sqrt), [rstd], [rstd])
                    k.V(lambda: nc.vector.reciprocal(out=rstd[:], in_=rstd[:]), [rstd], [rstd])
                    k.V(lambda: nc.vector.tensor_scalar(out=x1[:], in0=x1[:], scalar1=mv[:, 0:1], scalar2=rstd[:, 0:1], op0=ALU.subtract, op1=ALU.mult), [x1, mv, rstd], [x1])
                    k.V(lambda: nc.vector.tensor_tensor(out=x1[:], in0=x1[:], in1=g2[:], op=ALU.mult), [x1, g2], [x1])
                    k.V(lambda: nc.vector.tensor_tensor(out=x1[:], in0=x1[:], in1=b2[:], op=ALU.add), [x1, b2], [x1])
                    k.store(out, out.t.ap()[vc * TOK + blk * 128:vc * TOK + (blk + 1) * 128, :], x1, x1[:])
            k.es = es

        for vc in range(nvc):
            body(vc)
        _finish(nc, k, out)
    return nc


def _finish(nc, k, out):
    k.barrier()


def _consts():
    bf = ml_dtypes.bfloat16
    c = {}
    c["c_identb"] = np.eye(128, dtype=np.float32).astype(bf)
    c["c_identf"] = np.eye(128, dtype=np.float32)
    i = np.arange(128)[:, None]; j = np.arange(128)[None, :]
    nm = np.zeros((128, 2, 128), np.float32)
    nm[:, 0, :] = np.where(j <= i, 0.0, NEG)
    nm[:, 1, :] = np.where(i <= j, 0.0, NEG)
    c["c_negmask"] = nm.astype(bf)
    rs = np.zeros((128, 32), np.float32)
    for m in range(16):
        rs[m + 16, m] = 1.0
        rs[m, m + 16] = 1.0
    c["c_rsw"] = rs.astype(bf)
    invf = (np.float32(500000.0) ** (-np.arange(0, 32, 2, dtype=np.float32) / np.float32(32))).astype(np.float32)
    cf = np.zeros((32, 2), np.float32)
    cf[:, 0] = np.concatenate([invf, invf])
    cf[:, 1] = np.concatenate([-np.ones(16), np.ones(16)])
    c["c_invf"] = cf
    c["c_tri"] = (i < j).astype(np.float32)
    c["c_onesf"] = np.ones((128, 128), np.float32)
    c["c_onesb"] = np.ones((128, 128), np.float32).astype(bf)
    io = np.zeros((128, 2, NE), np.float32)
    io[:, 0, :] = np.arange(NE)[None, :]
    io[:, 1, :] = np.arange(NE)[None, :] * CAP
    c["c_iota"] = io
    return c


def _vbias(core):
    s = core * TOK
    vb = np.zeros((128, 69), np.float32)
    ci = 0
    for g, Dl in enumerate(DIL):
        nb = TOK // Dl // 128
        for r in range(Dl):
            for m in range(nb + 1):
                off = (HALO // Dl - 64 + 128 * m) * Dl + r
                xs = off + np.arange(128) * Dl
                glob = s - HALO + xs
                vb[:, ci] = np.where((glob >= 0) & (glob < SEQ), 0.0, NEG)
                ci += 1
    return vb


_NC_CACHE = {}
NCK = 8
NVC = NCORES // NCK


def make_in_map(x, positions, w_in, w_attn_out, sgu_ln_g, sgu_ln_b, sgu_w, sgu_b, w_sgu_out, w_out,
                ln1_g, ln1_b, w_router, b_router, w_gate_up, b_gate_up, w_down, b_down, ln2_g, ln2_b):
    f = np.float32
    x2 = np.asarray(x, f).reshape(SEQ, D)
    xpad = np.zeros((SEQ + 2 * HALO, D), f)
    xpad[HALO:HALO + SEQ] = x2
    ppad = np.zeros((1, SEQ + 2 * HALO), np.int32)
    ppad[0, HALO:HALO + SEQ] = np.asarray(positions, np.int32).reshape(SEQ)
    m = {
        "x_ext": xpad, "pos": ppad, "vbias": np.stack([_vbias(c) for c in range(NCORES)], 0),
        "w_in": np.asarray(w_in, f)[0], "w_ao": np.asarray(w_attn_out, f)[0], "w_so": np.asarray(w_sgu_out, f)[0],
        "w_o": np.asarray(w_out, f)[0],
        "w_gu": np.asarray(w_gate_up, f)[0].reshape(NE * D, 2 * DH), "w_d": np.asarray(w_down, f)[0].reshape(NE * DH, D),
        "sgu_g": np.asarray(sgu_ln_g, f).reshape(1, 1024), "sgu_bb": np.asarray(sgu_ln_b, f).reshape(1, 1024),
        "sgu_wT": np.ascontiguousarray(np.asarray(sgu_w, f)[0].transpose(2, 0, 1)),
        "sgu_bias": np.asarray(sgu_b, f).reshape(1, 1024),
        "ln1g": np.asarray(ln1_g, f).reshape(1, D), "ln1b": np.asarray(ln1_b, f).reshape(1, D),
        "ln2g": np.asarray(ln2_g, f).reshape(1, D), "ln2b": np.asarray(ln2_b, f).reshape(1, D),
        "w_r": np.ascontiguousarray(np.asarray(w_router, f)[0].reshape(KC, 128, NE).transpose(1, 0, 2)),
        "b_r": np.asarray(b_router, f).reshape(1, NE),
        "b_gu": np.ascontiguousarray(np.asarray(b_gate_up, f)[0].reshape(NE, 32, 128).transpose(2, 0, 1)),
        "b_d": np.asarray(b_down, f)[0],
        **_consts(),
    }
    return m


def kernel(**inputs):
    m = make_in_map(**inputs)
    span = NVC * TOK
    maps = []
    for c in range(NCK):
        mc = dict(m)
        mc["x_ext"] = m["x_ext"][c * span:c * span + span + 2 * HALO]
        mc["pos"] = np.ascontiguousarray(m["pos"][:, c * span:c * span + span + 2 * HALO])
        mc["vbias"] = np.ascontiguousarray(m["vbias"][c * NVC:(c + 1) * NVC])
        maps.append(mc)
    if "nc" not in _NC_CACHE:
        _NC_CACHE["nc"] = build_nc(nvc=NVC)
    res = run_bass_kernel_spmd(_NC_CACHE["nc"], maps, core_ids=list(range(NCK)))
    outp = np.concatenate([np.asarray(r["out"], np.float32) for r in res.results], axis=0)
    return outp.reshape(1, SEQ, D)
```

```python
import contextlib
import numpy as np
import ml_dtypes
import concourse.bass as bass
import concourse.mybir as mybir
from concourse.bass_utils import run_bass_kernel_spmd

F32, BF16, I32, U32 = mybir.dt.float32, mybir.dt.bfloat16, mybir.dt.int32, mybir.dt.uint32
AF = mybir.ActivationFunctionType
ALU = mybir.AluOpType
AX = mybir.AxisListType

NCORES = 8
D = 2048
SEQ = 16384
TOK = SEQ // NCORES
HALO = 1024
EXT = TOK + 2 * HALO
TT = 512
KC = D // 128
INW = 10752
NE = 32
NEL = NE // NCORES
CAP = 384
NSL = CAP // 128
DH = 2048
DIL = (1, 4, 16)
ALPHA = 2 ** 0.25
EPS = 1e-5
NEG = -30000.0
QOFF, KOFF, VOFF, ZOFF, GAOFF, GBOFF = 0, 1536, 3072, 4608, 6656, 8704


class Ctr:
    def __init__(self, nc, es, name, step):
        self.sem = es.enter_context(nc.semaphore(name))
        self.n = 0
        self.step = step
        self.inorder = False


class Stream:
    def __init__(self, eng, ctr):
        self.eng = eng
        self.ctr = ctr
        self.seen = {}


class Buf:
    def __init__(self, t):
        self.t = t
        self.w = None
        self.r = {}

    def __getitem__(self, k):
        return self.t[k]


class KB:
    def __init__(self, nc, es):
        self.nc, self.es = nc, es
        self.c_pe = Ctr(nc, es, "c_pe", 1)
        self.c_pe.inorder = True
        self.c_act = Ctr(nc, es, "c_act", 1)
        self.c_dve = Ctr(nc, es, "c_dve", 1)
        self.c_pool = Ctr(nc, es, "c_pool", 1)
        self.c_dl = Ctr(nc, es, "c_dl", 16)
        self.c_ds = Ctr(nc, es, "c_ds", 16)
        self.c_dg = Ctr(nc, es, "c_dg", 16)
        self.c_cc = Ctr(nc, es, "c_cc", 1)
        self.pe = Stream(nc.tensor, self.c_pe)
        self.act = Stream(nc.scalar, self.c_act)
        self.dve = Stream(nc.vector, self.c_dve)
        self.pool = Stream(nc.gpsimd, self.c_pool)
        self.sp = Stream(nc.sync, None)
        self.streams = [self.pe, self.act, self.dve, self.pool, self.sp]
        self.ctrs = [self.c_pe, self.c_act, self.c_dve, self.c_pool, self.c_dl, self.c_ds, self.c_dg, self.c_cc]
        self.nbuf = 0

    def sb(self, shape, dt, name=None):
        self.nbuf += 1
        return Buf(self.es.enter_context(self.nc.sbuf_tensor(f"{name or 'sb'}_{self.nbuf}", list(shape), dt)))

    def ps(self, shape, dt, name=None):
        self.nbuf += 1
        return Buf(self.es.enter_context(self.nc.psum_tensor(name or f"ps{self.nbuf}", list(shape), dt)))

    def dram(self, name, shape, dt):
        return Buf(self.nc.dram_tensor(name, list(shape), dt))

    def op(self, st, fn, reads=(), writes=(), ctr=None):
        ctr = ctr or st.ctr
        deps = {}

        def add(d):
            if d is not None:
                c, n = d
                if deps.get(c, 0) < n:
                    deps[c] = n
        for b in reads:
            add(b.w)
        for b in writes:
            add(b.w)
            for c, n in b.r.items():
                add((c, n))
        for c, n in deps.items():
            if c is st.ctr and c.inorder:
                continue
            if st.seen.get(c, 0) >= n:
                continue
            st.eng.wait_ge(c.sem, n)
            st.seen[c] = n
        inst = fn()
        ctr.n += ctr.step
        inst.then_inc(ctr.sem, ctr.step)
        for b in reads:
            if b.r.get(ctr, 0) < ctr.n:
                b.r[ctr] = ctr.n
        for b in writes:
            b.w = (ctr, ctr.n)
            b.r = {}
        return inst

    def barrier(self):
        for st in self.streams:
            for c in self.ctrs:
                if c.n > 0 and st.seen.get(c, 0) < c.n:
                    st.eng.wait_ge(c.sem, c.n)
                    st.seen[c] = c.n

    def mm(self, out, o_ap, lhsT, l_ap, rhs, r_ap, start, stop, extra_reads=()):
        return self.op(self.pe, lambda: self.nc.tensor.matmul(o_ap, lhsT=l_ap, rhs=r_ap, start=start, stop=stop),
                       reads=[lhsT, rhs, *extra_reads], writes=[out])

    def tr(self, out, o_ap, in_, i_ap, ident, id_ap):
        return self.op(self.pe, lambda: self.nc.tensor.transpose(o_ap, i_ap, id_ap), reads=[in_, ident], writes=[out])

    def load(self, out, o_ap, src, s_ap, **kw):
        return self.op(self.sp, lambda: self.nc.sync.dma_start(out=o_ap, in_=s_ap, **kw), reads=[src], writes=[out], ctr=self.c_dl)

    def store(self, out, o_ap, src, s_ap, **kw):
        return self.op(self.pool, lambda: self.nc.gpsimd.dma_start(out=o_ap, in_=s_ap, **kw), reads=[src], writes=[out], ctr=self.c_ds)

    def store_sp(self, out, o_ap, src, s_ap, **kw):
        return self.op(self.sp, lambda: self.nc.sync.dma_start(out=o_ap, in_=s_ap, **kw), reads=[src], writes=[out], ctr=self.c_dl)

    def gdma(self, out, o_ap, src, s_ap, **kw):
        return self.op(self.pool, lambda: self.nc.gpsimd.dma_start(out=o_ap, in_=s_ap, **kw), reads=[src], writes=[out], ctr=self.c_dg)

    def V(self, fn, reads, writes):
        return self.op(self.dve, fn, reads, writes)

    def A(self, fn, reads, writes):
        return self.op(self.act, fn, reads, writes)


def ss(off, step, n=128):
    return slice(off, off + (n - 1) * step + 1, step)


def bcast_rows(ap2d_tensor, row, c0, n, parts=128):
    t = ap2d_tensor
    ncols = t.shape[-1]
    return bass.AP(t, row * ncols + c0, [[0, parts], [1, n]])


def build_nc(stage=99, dbg=False, nvc=NCORES):
    nc = bass.Bass("TRN2", target_bir_lowering=False)

    def din(name, shape, dt=F32):
        return Buf(nc.dram_tensor(name, list(shape), dt, kind="ExternalInput"))

    x_ext = din("x_ext", [nvc * TOK + 2 * HALO, D])
    pos = din("pos", [1, nvc * TOK + 2 * HALO], I32)
    vbias = din("vbias", [nvc, 128, 69])
    w_in = din("w_in", [D, INW])
    w_ao = din("w_ao", [512, D])
    w_so = din("w_so", [1024, D])
    w_o = din("w_o", [D, D])
    if stage >= 4:
        w_gu = din("w_gu", [NE * D, 2 * DH])
        w_d = din("w_d", [NE * DH, D])
    sgu_g = din("sgu_g", [1, 1024])
    sgu_bb = din("sgu_bb", [1, 1024])
    sgu_wT = din("sgu_wT", [128, 8, 128])
    sgu_bias = din("sgu_bias", [1, 1024])
    ln1g = din("ln1g", [1, D]); ln1b = din("ln1b", [1, D])
    ln2g = din("ln2g", [1, D]); ln2b = din("ln2b", [1, D])
    w_r = din("w_r", [128, KC, NE])
    b_r = din("b_r", [1, NE])
    b_gu = din("b_gu", [128, NE, 32])
    b_d = din("b_d", [NE, D])
    c_identb = din("c_identb", [128, 128], BF16)
    c_identf = din("c_identf", [128, 128])
    c_negmask = din("c_negmask", [128, 2, 128], BF16)
    c_rsw = din("c_rsw", [128, 32], BF16)
    c_invf = din("c_invf", [32, 2])
    c_tri = din("c_tri", [128, 128])
    c_onesf = din("c_onesf", [128, 128])
    c_onesb = din("c_onesb", [128, 128], BF16)
    c_iota = din("c_iota", [128, 2, NE])

    out = Buf(nc.dram_tensor("out", [nvc * TOK, D], F32, kind="ExternalOutput"))
    if dbg:
        dbg_oa = Buf(nc.dram_tensor("dbg_oa", [4, 128, TOK], F32, kind="ExternalOutput"))
        dbg_x1 = Buf(nc.dram_tensor("dbg_x1", [TOK, D], F32, kind="ExternalOutput"))

    with contextlib.ExitStack() as es:
        k = KB(nc, es)
        win_g = k.dram("win_g", [D, INW], BF16)
        wao_g = k.dram("wao_g", [512, D], BF16)
        wso_g = k.dram("wso_g", [1024, D], BF16)
        wo_g = k.dram("wo_g", [D, D], BF16)
        xT_d = k.dram("xT_d", [KC, 128, EXT], BF16)
        cs_d = k.dram("cs_d", [2, 32, EXT], F32)
        mg_d = k.dram("mg_d", [KC, 128, TOK], BF16)
        ug_d = k.dram("ug_d", [8, 128, TOK], BF16)
        x1_d = k.dram("x1_d", [TOK, D], F32)
        x1b_d = k.dram("x1b_d", [TOK, D], BF16)
        xg_d = k.dram("xg_d", [NE * CAP, D], BF16)
        yg_d = k.dram("yg_d", [NE * CAP, D], F32)

        def cast(dst, src, rows, cols):
            c2 = max(c for c in range(1, 2049) if cols % c == 0)
            for r0 in range(0, rows, 2048):
                r1 = min(rows, r0 + 2048)
                k.gdma(dst, dst.t.ap()[r0:r1].rearrange("r (a c) -> r a c", c=c2),
                       src, src.t.ap()[r0:r1].rearrange("r (a c) -> r a c", c=c2))

        cast(win_g, w_in, D, INW)
        cast(wao_g, w_ao, 512, D)
        cast(wso_g, w_so, 1024, D)
        cast(wo_g, w_o, D, D)
        wsT = k.sb([128, 8, 128], BF16, "wsT")
        sb_row = k.sb([1, 1024], BF16, "sb_row")
        k.gdma(wsT, wsT[:], sgu_wT, sgu_wT.t.ap())
        k.gdma(sb_row, sb_row[:], sgu_bias, sgu_bias.t.ap())

        if stage >= 4:
            zt = k.sb([128, D], BF16, "zt")
            k.V(lambda: nc.vector.memset(zt[:], 0.0), [], [zt])
            for i in range(NE * CAP // 128):
                k.store(xg_d, xg_d.t.ap()[i * 128:(i + 1) * 128, :], zt, zt[:])
        identb = k.sb([128, 128], BF16, "identb"); identf = k.sb([128, 128], F32, "identf")
        onesb = k.sb([128, 128], BF16, "onesb"); onesf = k.sb([128, 128], F32, "onesf")
        k.load(identb, identb[:], c_identb, c_identb.t.ap())
        k.load(identf, identf[:], c_identf, c_identf.t.ap())
        k.load(onesb, onesb[:], c_onesb, c_onesb.t.ap())
        k.load(onesf, onesf[:], c_onesf, c_onesf.t.ap())
        psA = k.ps([128, 512], F32, "psA"); psB = k.ps([128, 512], F32, "psB")
        psC = k.ps([128, 512], F32, "psC"); psD = k.ps([128, 512], F32, "psD")
        psTb = k.ps([128, 1024], BF16, "psTb"); psTf = k.ps([128, 512], F32, "psTf")
        psS = k.ps([128, 512], F32, "psS"); psE = k.ps([128, 512], F32, "psE")

        rt_gate = k.sb([128, 16, 4], F32, "rt_gate")
        rt_slot = k.sb([128, 16, 4], I32, "rt_slot")

        def body(vc):
            es_oa = contextlib.ExitStack()
            k.es = es_oa
            oaT = k.sb([128, 4, TOK], BF16, "oaT")
            k.es = es
            if stage < 1:
                return
            with contextlib.ExitStack() as es1:
                k.es = es1
                xin = k.sb([128, D], F32, "xin")
                xtb = k.sb([128, KC, 128], BF16, "xtb")
                for tb in range(EXT // 128):
                    k.load(xin, xin[:], x_ext, x_ext.t.ap()[vc * TOK + tb * 128:vc * TOK + (tb + 1) * 128, :])
                    for kc4 in range(KC // 4):
                        for q in range(4):
                            kc = kc4 * 4 + q
                            k.tr(psTf, psTf[:, q * 128:(q + 1) * 128], xin, xin[:, kc * 128:(kc + 1) * 128], identf, identf[:])
                        k.V(lambda kc4=kc4: nc.vector.tensor_copy(out=xtb[:, kc4 * 4:(kc4 + 1) * 4, :],
                                                                   in_=psTf[:].rearrange("p (a b) -> p a b", b=128)),
                            reads=[psTf], writes=[xtb])
                    k.store(xT_d, xT_d.t.ap()[:, :, tb * 128:(tb + 1) * 128].rearrange("kc p t -> p kc t"), xtb, xtb[:])
                posi = k.sb([32, EXT], I32, "posi"); ang = k.sb([32, EXT], F32, "ang")
                t1 = k.sb([32, EXT], F32, "t1"); ni = k.sb([32, EXT], I32, "ni"); nf = k.sb([32, EXT], F32, "nf")
                invf = k.sb([32, 2], F32, "invf")
                k.load(invf, invf[:], c_invf, c_invf.t.ap())
                k.load(posi, posi[:], pos, bass.AP(pos.t, vc * TOK, [[0, 32], [1, EXT]]))
                k.V(lambda: nc.vector.tensor_copy(out=ang[:], in_=posi[:]), [posi], [ang])
                k.V(lambda: nc.vector.tensor_scalar(out=ang[:], in0=ang[:], scalar1=invf[:, 0:1], scalar2=None, op0=ALU.mult), [ang, invf], [ang])
                C1 = 6.28125
                C2 = float(2 * np.pi - C1)
                for which in range(2):
                    shift = float(np.pi / 2) if which == 0 else 0.0
                    k.V(lambda: nc.vector.tensor_scalar(out=t1[:], in0=ang[:], scalar1=shift, scalar2=float(1 / (2 * np.pi)), op0=ALU.add, op1=ALU.mult), [ang], [t1])
                    k.V(lambda: nc.vector.tensor_copy(out=ni[:], in_=t1[:]), [t1], [ni])
                    k.V(lambda: nc.vector.tensor_copy(out=nf[:], in_=ni[:]), [ni], [nf])
                    k.V(lambda: nc.vector.tensor_scalar(out=t1[:], in0=ang[:], scalar1=shift, scalar2=None, op0=ALU.add), [ang], [t1])
                    k.V(lambda: nc.vector.scalar_tensor_tensor(out=t1[:], in0=nf[:], scalar=-C1, in1=t1[:], op0=ALU.mult, op1=ALU.add), [nf, t1], [t1])
                    k.V(lambda: nc.vector.scalar_tensor_tensor(out=t1[:], in0=nf[:], scalar=-C2, in1=t1[:], op0=ALU.mult, op1=ALU.add), [nf, t1], [t1])
                    k.V(lambda: nc.vector.tensor_scalar(out=nf[:], in0=t1[:], scalar1=float(np.pi), scalar2=float(-2 * np.pi), op0=ALU.is_gt, op1=ALU.mult), [t1], [nf])
                    k.V(lambda: nc.vector.tensor_tensor(out=t1[:], in0=t1[:], in1=nf[:], op=ALU.add), [t1, nf], [t1])
                    k.V(lambda: nc.vector.tensor_scalar(out=nf[:], in0=t1[:], scalar1=float(-np.pi), scalar2=float(2 * np.pi), op0=ALU.is_lt, op1=ALU.mult), [t1], [nf])
                    k.V(lambda: nc.vector.tensor_tensor(out=t1[:], in0=t1[:], in1=nf[:], op=ALU.add), [t1, nf], [t1])
                    k.V(lambda: nc.vector.tensor_scalar(out=t1[:], in0=t1[:], scalar1=float(np.pi), scalar2=float(-np.pi), op0=ALU.min, op1=ALU.max), [t1], [t1])
                    k.A(lambda: nc.scalar.activation(out=nf[:], in_=t1[:], func=AF.Sin), [t1], [nf])
                    if which == 1:
                        k.V(lambda: nc.vector.tensor_scalar(out=nf[:], in0=nf[:], scalar1=invf[:, 1:2], scalar2=None, op0=ALU.mult), [nf, invf], [nf])
                    k.store(cs_d, cs_d.t.ap()[which], nf, nf[:])
                k.barrier()
            k.es = es

            if stage < 2:
                return
            with contextlib.ExitStack() as es2:
                k.es = es2
                negmask = k.sb([128, 2, 128], BF16, "negmask"); rsw = k.sb([128, 32], BF16, "rsw")
                vb = k.sb([128, 69], F32, "vb")
                k.load(negmask, negmask[:], c_negmask, c_negmask.t.ap())
                k.load(rsw, rsw[:], c_rsw, c_rsw.t.ap())
                k.load(vb, vb[:], vbias, vbias.t.ap()[vc])
                wp = k.sb([128, KC, 384], BF16, "wp")
                xts = [k.sb([128, KC, TT], BF16, f"xt{i}") for i in range(2)]
                csts = [k.sb([32, 2, TT], F32, f"cst{i}") for i in range(2)]
                pproj = [psA, psD]
                rot = {"x": 0, "p": 0, "b": 0}
                bufA = k.sb([128, 3, EXT], BF16, "bufA")
                vt = k.sb([128, 69, 128], BF16, "vt")
                qT = k.sb([128, 3, TOK], BF16, "qT")
                acc = k.sb([128, 2, TOK], F32, "acc")
                pTs = [k.sb([128, 2, 128], BF16, f"pT{i}") for i in range(2)]
                r1 = k.sb([32, TT], F32, "r1"); r2 = k.sb([32, TT], F32, "r2")
                rden = k.sb([128, TOK], F32, "rden")
                kt_idx = {}
                for g, Dl in enumerate(DIL):
                    nb = TOK // Dl // 128
                    for r in range(Dl):
                        for m in range(nb + 1):
                            kt_idx[(g, r, m)] = len(kt_idx)
                assert len(kt_idx) == 69

                def proj(dst, width, col0, tiles, rope):
                    pass

                for h in range(4):
                    for kind in ("v", "k", "q"):
                        base = {"q": QOFF, "k": KOFF, "v": VOFF}[kind]
                        for g in range(3):
                            c0 = base + g * 512 + h * 128
                            k.load(wp, wp[:, :, g * 128:(g + 1) * 128], win_g,
                                   win_g.t.ap()[:, c0:c0 + 128].rearrange("(kc p) c -> p kc c", p=128))
                        tiles = range(2, 6) if kind == "q" else range(0, 8)
                        dst = qT if kind == "q" else bufA

                        def rope_stage(dst, g, d0, cst):
                            dap = dst[:, g, d0:d0 + TT]
                            k.mm(psB, psB[0:32, :], rsw, rsw[:], dst, dap, True, True)
                            k.V(lambda: nc.vector.tensor_tensor(out=r1[:], in0=psB[0:32, :], in1=cst[:, 1, :], op=ALU.mult), [psB, cst], [r1])
                            k.V(lambda: nc.vector.tensor_tensor(out=r2[:], in0=dst[0:32, g, d0:d0 + TT], in1=cst[:, 0, :], op=ALU.mult), [dst, cst], [r2])
                            k.V(lambda: nc.vector.tensor_tensor(out=dst[0:32, g, d0:d0 + TT], in0=r1[:], in1=r2[:], op=ALU.add), [r1, r2], [dst])

                        pending = None
                        for ti in tiles:
                            t0 = ti * TT
                            xt = xts[rot["x"] % 2]
                            cst = csts[rot["x"] % 2]
                            rot["x"] += 1
                            k.load(xt, xt[:], xT_d, xT_d.t.ap()[:, :, t0:t0 + TT].rearrange("kc p t -> p kc t"))
                            if kind != "v":
                                k.load(cst, cst[:], cs_d, cs_d.t.ap()[:, :, t0:t0 + TT].rearrange("w f t -> f w t"))
                            for g in range(3):
                                if kind != "q" and g < 2 and (ti < 1 or ti > 6):
                                    continue
                                pp = pproj[rot["p"] % 2]
                                rot["p"] += 1
                                for kc in range(KC):
                                    k.mm(pp, pp[:], wp, wp[:, kc, g * 128:(g + 1) * 128], xt, xt[:, kc, :], kc == 0, kc == KC - 1)
                                d0 = t0 - (HALO if kind == "q" else 0)
                                dap = dst[:, g, d0:d0 + TT]
                                k.A(lambda dap=dap, pp=pp: nc.scalar.activation(out=dap, in_=pp[:], func=AF.Copy), [pp], [dst])
                                if kind != "v":
                                    if pending is not None:
                                        rope_stage(*pending)
                                    pending = (dst, g, d0, cst)
                        if pending is not None:
                            rope_stage(*pending)
                        if kind == "v":
                            for (g, r, m), ci in kt_idx.items():
                                Dl = DIL[g]
                                off = (HALO // Dl - 64 + 128 * m) * Dl + r
                                src = bufA[:, g, ss(off, Dl)]
                                j = ci % 8
                                k.tr(psTb, psTb[:, j * 128:(j + 1) * 128], bufA, src, identb, identb[:])
                                if j == 7 or ci == 68:
                                    nj = j + 1
                                    c00 = ci - j
                                    k.V(lambda nj=nj, c00=c00: nc.vector.tensor_copy(
                                        out=vt[:, c00:c00 + nj, :], in_=psTb[:, 0:nj * 128].rearrange("p (a b) -> p a b", b=128)),
                                        [psTb], [vt])
                    pss = [psS, psE]
                    pcs = [psC, psTf]

                    def pv_stage(pT, pc, g, r, b, qoff, Dl):
                        for half in range(2):
                            ci = kt_idx[(g, r, b + half)]
                            k.mm(pc, pc[:, 0:128], vt, vt[:, ci, :], pT, pT[:, half, :], half == 0, half == 1)
                        for half in range(2):
                            k.mm(pc, pc[:, 128:256], onesb, onesb[:], pT, pT[:, half, :], half == 0, half == 1)
                        aap = acc[:, :, ss(qoff, Dl)]
                        pap = pc[:, 0:256].rearrange("p (a b) -> p a b", b=128)
                        if g == 0:
                            k.V(lambda: nc.vector.tensor_copy(out=aap, in_=pap), [pc], [acc])
                        else:
                            k.V(lambda: nc.vector.tensor_tensor(out=aap, in0=pap, in1=aap, op=ALU.add), [pc, acc], [acc])

                    pend = None
                    for g, Dl in enumerate(DIL):
                        nb = TOK // Dl // 128
                        for r in range(Dl):
                            for b in range(nb):
                                bi = rot["b"]
                                rot["b"] += 1
                                psc = pss[bi % 2]
                                pT = pTs[bi % 2]
                                qoff = (b * 128) * Dl + r
                                qap = qT[:, g, ss(qoff, Dl)]
                                for half in range(2):
                                    m = b + half
                                    koff = (HALO // Dl - 64 + 128 * m) * Dl + r
                                    kap = bufA[:, g, ss(koff, Dl)]
                                    so = psc[:, half * 128:(half + 1) * 128]
                                    k.mm(psc, so, bufA, kap, qT, qap, True, False)
                                    k.mm(psc, so, identb, identb[:], negmask, negmask[:, half, :], False, True)
                                    ci = kt_idx[(g, r, m)]
                                    k.A(lambda so=so, half=half, ci=ci, pT=pT: nc.scalar.activation(
                                        out=pT[:, half, :], in_=so, func=AF.Exp, bias=vb[:, ci:ci + 1], scale=float(128 ** -0.5)),
                                        [psc, vb], [pT])
                                if pend is not None:
                                    pv_stage(*pend)
                                pend = (pT, pcs[bi % 2], g, r, b, qoff, Dl)
                    if pend is not None:
                        pv_stage(*pend)
                    k.V(lambda: nc.vector.reciprocal(out=rden[:], in_=acc[:, 1, :]), [acc], [rden])
                    k.V(lambda: nc.vector.tensor_tensor(out=acc[:, 0, :], in0=acc[:, 0, :], in1=rden[:], op=ALU.mult), [acc, rden], [acc])
                    k.V(lambda h=h: nc.vector.tensor_copy(out=oaT[:, h, :], in_=acc[:, 0, :]), [acc], [oaT])
                    if dbg and vc == 0:
                        k.store(dbg_oa, dbg_oa.t.ap()[h], acc, acc[:, 0, :])
                k.barrier()
            k.es = es
            if stage < 3:
                return

            with contextlib.ExitStack() as es3:
                k.es = es3
                wzs = [k.sb([128, KC, 512], BF16, f"wz{i}") for i in range(2)]
                wzi = [0]
                xt = k.sb([128, KC, TT], BF16, "xt3")
                uT = k.sb([128, 8, TT], BF16, "uT")
                vact = k.sb([128, 1024], F32, "vact")
                vall = k.sb([128, 4, 1024], F32, "vall")
                vn = k.sb([128, 4, 1024], BF16, "vn")
                gB = k.sb([128, 1024], F32, "gB"); bB = k.sb([128, 1024], F32, "bB")
                st6 = k.sb([128, 2, 6], F32, "st6"); mv = k.sb([128, 2], F32, "mv"); rstd = k.sb([128, 1], F32, "rstd")
                ugt = k.sb([128, TT], BF16, "ugt")
                k.load(gB, gB[:], sgu_g, bcast_rows(sgu_g.t, 0, 0, 1024))
                k.load(bB, bB[:], sgu_bb, bcast_rows(sgu_bb.t, 0, 0, 1024))
                for ti in range(4):
                    t0 = HALO + ti * TT
                    k.load(xt, xt[:], xT_d, xT_d.t.ap()[:, :, t0:t0 + TT].rearrange("kc p t -> p kc t"))
                    for half in range(2):
                        c0 = ZOFF + half * 512
                        wz = wzs[wzi[0] % 2]
                        wzi[0] += 1
                        k.load(wz, wz[:], win_g, win_g.t.ap()[:, c0:c0 + 512].rearrange("(kc p) c -> p kc c", p=128))
                        for j in range(4):
                            for kc in range(KC):
                                k.mm(psA, psA[:], wz, wz[:, kc, j * 128:(j + 1) * 128], xt, xt[:, kc, :], kc == 0, kc == KC - 1)
                            k.A(lambda half=half, j=j: nc.scalar.activation(out=uT[:, half * 4 + j, :], in_=psA[:], func=AF.Gelu), [psA], [uT])
                    for half in range(2):
                        c0 = ZOFF + 1024 + half * 512
                        wz = wzs[wzi[0] % 2]
                        wzi[0] += 1
                        k.load(wz, wz[:], win_g, win_g.t.ap()[:, c0:c0 + 512].rearrange("(kc p) c -> p kc c", p=128))
                        for sblk in range(4):
                            for kc in range(KC):
                                k.mm(psA, psA[:], xt, xt[:, kc, sblk * 128:(sblk + 1) * 128], wz, wz[:, kc, :], kc == 0, kc == KC - 1)
                            k.A(lambda half=half, sblk=sblk: nc.scalar.activation(out=vall[:, sblk, half * 512:(half + 1) * 512], in_=psA[:], func=AF.Gelu), [psA], [vall])
                    for sblk in range(4):
                        for half in range(2):
                            k.V(lambda half=half, sblk=sblk: nc.vector.bn_stats(out=st6[:, half, :], in_=vall[:, sblk, half * 512:(half + 1) * 512]), [vall], [st6])
                        k.V(lambda: nc.vector.bn_aggr(out=mv[:], in_=st6[:].rearrange("p a b -> p (a b)")), [st6], [mv])
                        k.V(lambda: nc.vector.tensor_scalar(out=rstd[:], in0=mv[:, 1:2], scalar1=EPS, scalar2=None, op0=ALU.add), [mv], [rstd])
                        k.A(lambda: nc.scalar.activation(out=rstd[:], in_=rstd[:], func=AF.Sqrt), [rstd], [rstd])
                        k.V(lambda: nc.vector.reciprocal(out=rstd[:], in_=rstd[:]), [rstd], [rstd])
                        k.V(lambda sblk=sblk: nc.vector.tensor_scalar(out=vact[:], in0=vall[:, sblk, :], scalar1=mv[:, 0:1], scalar2=rstd[:, 0:1], op0=ALU.subtract, op1=ALU.mult), [vall, mv, rstd], [vact])
                        k.V(lambda: nc.vector.tensor_tensor(out=vact[:], in0=vact[:], in1=gB[:], op=ALU.mult), [vact, gB], [vact])
                        k.V(lambda sblk=sblk: nc.vector.tensor_tensor(out=vn[:, sblk, :], in0=vact[:], in1=bB[:], op=ALU.add), [vact, bB], [vn])
                    for g in range(8):
                        for sblk in range(4):
                            o = psB[:, sblk * 128:(sblk + 1) * 128]
                            k.mm(psB, o, vn, vn[:, sblk, g * 128:(g + 1) * 128], wsT, wsT[:, g, :], True, False)
                            k.mm(psB, o, onesb, onesb[0:1, :], sb_row, sb_row[0:1, g * 128:(g + 1) * 128], False, True)
                        k.V(lambda g=g: nc.vector.tensor_tensor(out=ugt[:], in0=psB[:], in1=uT[:, g, :], op=ALU.mult), [psB, uT], [ugt])
                        k.store(ug_d, ug_d.t.ap()[g, :, ti * TT:(ti + 1) * TT], ugt, ugt[:])
                k.barrier()
            k.es = es

            with contextlib.ExitStack() as es3:
                k.es = es3
                xto = k.sb([128, KC, TOK], BF16, "xto")
                ugT = k.sb([128, 8, TOK], BF16, "ugT")
                wgas = [k.sb([128, KC, 128], BF16, f"wga{i}") for i in range(2)]; wgbs = [k.sb([128, KC, 128], BF16, f"wgb{i}") for i in range(2)]
                was = [k.sb([128, 4, 128], BF16, f"wa{i}") for i in range(2)]; wss = [k.sb([128, 8, 128], BF16, f"ws{i}") for i in range(2)]
                sa = k.sb([128, TT], F32, "sa"); sbg = k.sb([128, TT], F32, "sbg")
                m1 = k.sb([128, TT], F32, "m1"); m2 = k.sb([128, TT], F32, "m2"); mgt = k.sb([128, TT], BF16, "mgt")
                k.load(xto, xto[:], xT_d, xT_d.t.ap()[:, :, HALO:HALO + TOK].rearrange("kc p t -> p kc t"))
                k.load(ugT, ugT[:], ug_d, ug_d.t.ap().rearrange("g p t -> p g t"))
                for j in range(KC):
                    wga, wgb, wa, ws = wgas[j % 2], wgbs[j % 2], was[j % 2], wss[j % 2]
                    k.load(wga, wga[:], win_g, win_g.t.ap()[:, GAOFF + j * 128:GAOFF + (j + 1) * 128].rearrange("(kc p) c -> p kc c", p=128))
                    k.load(wgb, wgb[:], win_g, win_g.t.ap()[:, GBOFF + j * 128:GBOFF + (j + 1) * 128].rearrange("(kc p) c -> p kc c", p=128))
                    k.load(wa, wa[:], wao_g, wao_g.t.ap()[:, j * 128:(j + 1) * 128].rearrange("(kc p) c -> p kc c", p=128))
                    k.load(ws, ws[:], wso_g, wso_g.t.ap()[:, j * 128:(j + 1) * 128].rearrange("(kc p) c -> p kc c", p=128))
                    for ti in range(4):
                        ts = slice(ti * TT, (ti + 1) * TT)
                        for kc in range(KC):
                            k.mm(psA, psA[:], wga, wga[:, kc, :], xto, xto[:, kc, ts], kc == 0, kc == KC - 1)
                        k.A(lambda: nc.scalar.activation(out=sa[:], in_=psA[:], func=AF.Sigmoid), [psA], [sa])
                        for kc in range(KC):
                            k.mm(psB, psB[:], wgb, wgb[:, kc, :], xto, xto[:, kc, ts], kc == 0, kc == KC - 1)
                        k.A(lambda: nc.scalar.activation(out=sbg[:], in_=psB[:], func=AF.Sigmoid), [psB], [sbg])
                        for kc in range(4):
                            k.mm(psC, psC[:], wa, wa[:, kc, :], oaT, oaT[:, kc, ts], kc == 0, kc == 3)
                        for kc in range(8):
                            k.mm(psD, psD[:], ws, ws[:, kc, :], ugT, ugT[:, kc, ts], kc == 0, kc == 7)
                        k.V(lambda: nc.vector.tensor_tensor(out=m1[:], in0=psC[:], in1=sa[:], op=ALU.mult), [psC, sa], [m1])
                        k.V(lambda: nc.vector.tensor_tensor(out=m2[:], in0=psD[:], in1=sbg[:], op=ALU.mult), [psD, sbg], [m2])
                        k.V(lambda: nc.vector.tensor_tensor(out=mgt[:], in0=m1[:], in1=m2[:], op=ALU.add), [m1, m2], [mgt])
                        k.store(mg_d, mg_d.t.ap()[j, :, ts], mgt, mgt[:])
                k.barrier()
            k.es = es

            es_oa.close()
            with contextlib.ExitStack() as es3:
                k.es = es3
                wo = k.sb([128, KC, D], BF16, "wo")
                mts = [k.sb([128, KC, 128], BF16, f"mt{i}") for i in range(2)]
                xrs = [k.sb([128, D], F32, f"xr{i}") for i in range(2)]
                hs = k.sb([128, D], F32, "hs")
                hb = k.sb([128, D], BF16, "hb")
                g1 = k.sb([128, D], F32, "g1"); b1 = k.sb([128, D], F32, "b1")
                st6 = k.sb([128, 4, 6], F32, "st6c"); mv = k.sb([128, 2], F32, "mvc"); rstd = k.sb([128, 1], F32, "rstdc")
                x1T = k.sb([128, KC, 128], F32, "x1T")
                wr = k.sb([128, KC, NE], F32, "wr"); brB = k.sb([128, NE], F32, "brB")
                lg = k.sb([128, NE], F32, "lg"); m8 = k.sb([128, 8], F32, "m8"); i8 = k.sb([128, 8], U32, "i8")
                i8f = k.sb([128, 8], F32, "i8f")
                msk = k.sb([128, 16, NE], F32, "msk")
                ex = k.sb([128, 8], F32, "ex"); ssum = k.sb([128, 1], F32, "ssum"); nmax = k.sb([128, 1], F32, "nmax")
                tri = k.sb([128, 128], F32, "tri")
                iot = k.sb([128, 2, NE], F32, "iot")
                rk = k.sb([128, NE], F32, "rk"); oh = k.sb([128, NE], F32, "oh"); sl = k.sb([128, 4], F32, "sl")
                k.load(wo, wo[:], wo_g, wo_g.t.ap().rearrange("(kc p) c -> p kc c", p=128))
                k.load(g1, g1[:], ln1g, bcast_rows(ln1g.t, 0, 0, D)); k.load(b1, b1[:], ln1b, bcast_rows(ln1b.t, 0, 0, D))
                k.load(wr, wr[:], w_r, w_r.t.ap()); k.load(brB, brB[:], b_r, bcast_rows(b_r.t, 0, 0, NE))
                k.load(tri, tri[:], c_tri, c_tri.t.ap()); k.load(iot, iot[:], c_iota, c_iota.t.ap())
                for blk in range(16):
                    tsl = slice(blk * 128, (blk + 1) * 128)
                    mt, xr = mts[blk % 2], xrs[blk % 2]
                    k.load(mt, mt[:], mg_d, mg_d.t.ap()[:, :, tsl].rearrange("kc p t -> p kc t"))
                    k.load(xr, xr[:], x_ext, x_ext.t.ap()[vc * TOK + HALO + blk * 128:vc * TOK + HALO + (blk + 1) * 128, :])
                    for fb, pst in enumerate((psA, psB, psC, psD)):
                        for kc in range(KC):
                            k.mm(pst, pst[:], mt, mt[:, kc, :], wo, wo[:, kc, fb * 512:(fb + 1) * 512], kc == 0, kc == KC - 1)
                        k.V(lambda fb=fb, pst=pst: nc.vector.scalar_tensor_tensor(
                            out=hs[:, fb * 512:(fb + 1) * 512], in0=xr[:, fb * 512:(fb + 1) * 512], scalar=float(ALPHA), in1=pst[:],
                            op0=ALU.mult, op1=ALU.add), [xr, pst], [hs])
                        k.V(lambda fb=fb: nc.vector.bn_stats(out=st6[:, fb, :], in_=hs[:, fb * 512:(fb + 1) * 512]), [hs], [st6])
                    k.V(lambda: nc.vector.bn_aggr(out=mv[:], in_=st6[:].rearrange("p a b -> p (a b)")), [st6], [mv])
                    k.V(lambda: nc.vector.tensor_scalar(out=rstd[:], in0=mv[:, 1:2], scalar1=EPS, scalar2=None, op0=ALU.add), [mv], [rstd])
                    k.A(lambda: nc.scalar.activation(out=rstd[:], in_=rstd[:], func=AF.Sqrt), [rstd], [rstd])
                    k.V(lambda: nc.vector.reciprocal(out=rstd[:], in_=rstd[:]), [rstd], [rstd])
                    k.V(lambda: nc.vector.tensor_scalar(out=hs[:], in0=hs[:], scalar1=mv[:, 0:1], scalar2=rstd[:, 0:1], op0=ALU.subtract, op1=ALU.mult), [hs, mv, rstd], [hs])
                    k.V(lambda: nc.vector.tensor_tensor(out=hs[:], in0=hs[:], in1=g1[:], op=ALU.mult), [hs, g1], [hs])
                    k.V(lambda: nc.vector.tensor_tensor(out=hs[:], in0=hs[:], in1=b1[:], op=ALU.add), [hs, b1], [hs])
                    k.A(lambda: nc.scalar.activation(out=hb[:], in_=hs[:], func=AF.Copy), [hs], [hb])
                    k.store(x1_d, x1_d.t.ap()[tsl, :], hs, hs[:])
                    k.store(x1b_d, x1b_d.t.ap()[tsl, :], hb, hb[:])
                    if dbg and vc == 0:
                        k.store(dbg_x1, dbg_x1.t.ap()[tsl, :], hs, hs[:])
                    if stage < 4:
                        continue
                    for kc4 in range(4):
                        for q in range(4):
                            kc = kc4 * 4 + q
                            k.tr(psTf, psTf[:, q * 128:(q + 1) * 128], hs, hs[:, kc * 128:(kc + 1) * 128], identf, identf[:])
                        k.V(lambda kc4=kc4: nc.vector.tensor_copy(out=x1T[:, kc4 * 4:(kc4 + 1) * 4, :], in_=psTf[:].rearrange("p (a b) -> p a b", b=128)), [psTf], [x1T])
                    for kc in range(KC):
                        k.mm(psS, psS[:, 0:NE], x1T, x1T[:, kc, :], wr, wr[:, kc, :], kc == 0, kc == KC - 1)
                    k.V(lambda: nc.vector.tensor_tensor(out=lg[:], in0=psS[:, 0:NE], in1=brB[:], op=ALU.add), [psS, brB], [lg])
                    k.V(lambda: nc.vector.max(out=m8[:], in_=lg[:]), [lg], [m8])
                    k.V(lambda: nc.vector.max_index(out=i8[:], in_max=m8[:], in_values=lg[:]), [m8, lg], [i8])
                    k.V(lambda: nc.vector.tensor_copy(out=i8f[:], in_=i8[:]), [i8], [i8f])
                    k.V(lambda blk=blk: nc.vector.tensor_scalar(out=msk[:, blk, :], in0=lg[:], scalar1=m8[:, 3:4], scalar2=None, op0=ALU.is_ge), [lg, m8], [msk])
                    k.V(lambda: nc.vector.tensor_scalar(out=nmax[:], in0=m8[:, 0:1], scalar1=-1.0, scalar2=None, op0=ALU.mult), [m8], [nmax])
                    k.A(lambda: nc.scalar.activation(out=ex[:, 0:4], in_=m8[:, 0:4], func=AF.Exp, bias=nmax[:, 0:1], scale=1.0), [m8, nmax], [ex])
                    k.V(lambda: nc.vector.tensor_reduce(out=ssum[:], in_=ex[:, 0:4], axis=AX.X, op=ALU.add), [ex], [ssum])
                    k.V(lambda: nc.vector.reciprocal(out=ssum[:], in_=ssum[:]), [ssum], [ssum])
                    k.V(lambda blk=blk: nc.vector.tensor_scalar(out=rt_gate[:, blk, :], in0=ex[:, 0:4], scalar1=ssum[:, 0:1], scalar2=None, op0=ALU.mult), [ex, ssum], [rt_gate])
                    k.mm(psE, psE[:, 0:NE], tri, tri[:], msk, msk[:, blk, :], True, blk == 0)
                    for pb in range(blk):
                        k.mm(psE, psE[:, 0:NE], onesf, onesf[:], msk, msk[:, pb, :], False, pb == blk - 1)
                    k.V(lambda: nc.vector.tensor_tensor(out=rk[:], in0=psE[:, 0:NE], in1=iot[:, 1, :], op=ALU.add), [psE, iot], [rk])
                    for j in range(4):
                        k.V(lambda j=j: nc.vector.tensor_scalar(out=oh[:], in0=iot[:, 0, :], scalar1=i8f[:, j:j + 1], scalar2=None, op0=ALU.is_equal), [iot, i8f], [oh])
                        k.V(lambda: nc.vector.tensor_tensor(out=oh[:], in0=oh[:], in1=rk[:], op=ALU.mult), [oh, rk], [oh])
                        k.V(lambda j=j: nc.vector.tensor_reduce(out=sl[:, j:j + 1], in_=oh[:], axis=AX.X, op=ALU.add), [oh], [sl])
                    k.V(lambda blk=blk: nc.vector.tensor_copy(out=rt_slot[:, blk, :], in_=sl[:]), [sl], [rt_slot])
                k.barrier()
            k.es = es
            if stage < 4:
                return

            with contextlib.ExitStack() as es4d:
                k.es = es4d
                xb = k.sb([128, D], BF16, "xb4")
                for blk in range(16):
                    k.load(xb, xb[:], x1b_d, x1b_d.t.ap()[blk * 128:(blk + 1) * 128, :])
                    for j in range(4):
                        k.op(k.pool, lambda blk=blk, j=j: nc.gpsimd.indirect_dma_start(
                            out=xg_d.t.ap(), out_offset=bass.IndirectOffsetOnAxis(ap=rt_slot[:, blk, j:j + 1], axis=0),
                            in_=xb[:], in_offset=None), reads=[xb, rt_slot], writes=[xg_d], ctr=k.c_dg)
                k.barrier()
            k.es = es
            with contextlib.ExitStack() as es4:
                k.es = es4
                NW = 4
                stg = [k.sb([128, KC, 512], F32, f"stg{i}") for i in range(2)]
                wbf = [k.sb([128, KC, 512], BF16, f"wbf{i}") for i in range(NW)]
                xgt = k.sb([128, NSL, D], BF16, "xgt")
                xgTs = [k.sb([128, KC, CAP], BF16, f"xgT{i}") for i in range(2)]
                hT = k.sb([128, KC, CAP], BF16, "hT")
                bgu = k.sb([128, NE, 32], F32, "bgu")
                tmp = [[k.sb([128, CAP], F32, f"t{n}{i}") for n in ("gc", "sg", "uc")] for i in range(2)]
                bdBs = [k.sb([128, 512], F32, f"bdB{i}") for i in range(2)]
                ysts = [k.sb([128, 512], F32, f"yst{i}") for i in range(2)]
                k.load(bgu, bgu[:], b_gu, b_gu.t.ap())
                wcnt = [0]

                def wload(src, ap):
                    i = wcnt[0]
                    wcnt[0] += 1
                    sgb = stg[i % 2]
                    w = wbf[i % NW]
                    k.load(sgb, sgb[:], src, ap)
                    k.A(lambda: nc.scalar.activation(out=w[:], in_=sgb[:], func=AF.Copy), [sgb], [w])
                    return w

                pgu = [(psA, psB), (psC, psD)]
                pdn = [psS, psE]
                hcn = 0
                dn = 0
                blocks = []
                for e in range(NE):
                    for J in range(4):
                        blocks.append((w_gu, w_gu.t.ap()[e * D:(e + 1) * D, J * 512:(J + 1) * 512].rearrange("(kc p) c -> p kc c", p=128)))
                        blocks.append((w_gu, w_gu.t.ap()[e * D:(e + 1) * D, DH + J * 512:DH + (J + 1) * 512].rearrange("(kc p) c -> p kc c", p=128)))
                    for F in range(4):
                        blocks.append((w_d, w_d.t.ap()[e * DH:(e + 1) * DH, F * 512:(F + 1) * 512].rearrange("(kc p) c -> p kc c", p=128)))
                wbl = {}

                def ensure(n):
                    while wcnt[0] < min(n, len(blocks)):
                        i = wcnt[0]
                        wbl[i] = wload(*blocks[i])
                bidx = 0
                for e in range(NE):
                    xgT = xgTs[e % 2]
                    k.load(xgt, xgt[:], xg_d, xg_d.t.ap()[e * CAP:(e + 1) * CAP, :].rearrange("(s p) c -> p s c", p=128))
                    for s in range(NSL):
                        for kc8 in range(2):
                            for q in range(8):
                                kc = kc8 * 8 + q
                                k.tr(psTb, psTb[:, q * 128:(q + 1) * 128], xgt, xgt[:, s, kc * 128:(kc + 1) * 128], identb, identb[:])
                            k.V(lambda s=s, kc8=kc8, xgT=xgT: nc.vector.tensor_copy(out=xgT[:, kc8 * 8:(kc8 + 1) * 8, s * 128:(s + 1) * 128],
                                                                                     in_=psTb[:].rearrange("p (a b) -> p a b", b=128)), [psTb], [xgT])
                    for J in range(4):
                        ensure(bidx + 4)
                        wg = wbl.pop(bidx)
                        wu = wbl.pop(bidx + 1)
                        bidx += 2
                        for jj in range(4):
                            hc = J * 4 + jj
                            pg, pu = pgu[hcn % 2]
                            gc, sg, uc = tmp[hcn % 2]
                            hcn += 1
                            for kc in range(KC):
                                k.mm(pg, pg[:, 0:CAP], wg, wg[:, kc, jj * 128:(jj + 1) * 128], xgT, xgT[:, kc, :], kc == 0, kc == KC - 1)
                            for kc in range(KC):
                                k.mm(pu, pu[:, 0:CAP], wu, wu[:, kc, jj * 128:(jj + 1) * 128], xgT, xgT[:, kc, :], kc == 0, kc == KC - 1)
                            k.V(lambda hc=hc, e=e, pg=pg, gc=gc: nc.vector.tensor_scalar(out=gc[:], in0=pg[:, 0:CAP], scalar1=bgu[:, e, hc:hc + 1], scalar2=7.0, op0=ALU.add, op1=ALU.min), [pg, bgu], [gc])
                            k.A(lambda gc=gc, sg=sg: nc.scalar.activation(out=sg[:], in_=gc[:], func=AF.Sigmoid, scale=1.702), [gc], [sg])
                            k.V(lambda hc=hc, e=e, pu=pu, uc=uc: nc.vector.tensor_scalar(out=uc[:], in0=pu[:, 0:CAP], scalar1=bgu[:, e, 16 + hc:16 + hc + 1], scalar2=7.0, op0=ALU.add, op1=ALU.min), [pu, bgu], [uc])
                            k.V(lambda uc=uc: nc.vector.tensor_scalar(out=uc[:], in0=uc[:], scalar1=-7.0, scalar2=1.0, op0=ALU.max, op1=ALU.add), [uc], [uc])
                            k.V(lambda gc=gc, sg=sg: nc.vector.tensor_tensor(out=gc[:], in0=gc[:], in1=sg[:], op=ALU.mult), [gc, sg], [gc])
                            k.V(lambda hc=hc, gc=gc, uc=uc: nc.vector.tensor_tensor(out=hT[:, hc, :], in0=gc[:], in1=uc[:], op=ALU.mult), [gc, uc], [hT])
                    for F in range(4):
                        ensure(bidx + 3)
                        wd = wbl.pop(bidx)
                        bidx += 1
                        bdB = bdBs[F % 2]
                        k.gdma(bdB, bdB[:], b_d, bcast_rows(b_d.t, e, F * 512, 512))
                        for s in range(NSL):
                            pd = pdn[dn % 2]
                            yst = ysts[dn % 2]
                            dn += 1
                            for kc in range(KC):
                                k.mm(pd, pd[:], hT, hT[:, kc, s * 128:(s + 1) * 128], wd, wd[:, kc, :], kc == 0, kc == KC - 1)
                            k.V(lambda pd=pd, yst=yst, bdB=bdB: nc.vector.tensor_tensor(out=yst[:], in0=pd[:], in1=bdB[:], op=ALU.add), [pd, bdB], [yst])
                            k.store(yg_d, yg_d.t.ap()[e * CAP + s * 128:e * CAP + (s + 1) * 128, F * 512:(F + 1) * 512], yst, yst[:])
                k.barrier()
            k.es = es

            with contextlib.ExitStack() as es5:
                k.es = es5
                x1s = [k.sb([128, D], F32, f"x1r{i}") for i in range(2)]
                yjs = [k.sb([128, D], F32, f"yj{i}") for i in range(4)]
                g2 = k.sb([128, D], F32, "g2"); b2 = k.sb([128, D], F32, "b2")
                st6 = k.sb([128, 4, 6], F32, "st6e"); mv = k.sb([128, 2], F32, "mve"); rstd = k.sb([128, 1], F32, "rstde")
                k.load(g2, g2[:], ln2g, bcast_rows(ln2g.t, 0, 0, D)); k.load(b2, b2[:], ln2b, bcast_rows(ln2b.t, 0, 0, D))
                for blk in range(16):
                    tsl = slice(blk * 128, (blk + 1) * 128)
                    x1 = x1s[blk % 2]
                    k.load(x1, x1[:], x1_d, x1_d.t.ap()[tsl, :])
                    k.V(lambda: nc.vector.tensor_scalar(out=x1[:], in0=x1[:], scalar1=float(ALPHA), scalar2=None, op0=ALU.mult), [x1], [x1])
                    for j in range(4):
                        yj = yjs[j]
                        k.op(k.pool, lambda blk=blk, j=j, yj=yj: nc.gpsimd.indirect_dma_start(
                            out=yj[:], out_offset=None, in_=yg_d.t.ap(),
                            in_offset=bass.IndirectOffsetOnAxis(ap=rt_slot[:, blk, j:j + 1], axis=0)),
                            reads=[yg_d, rt_slot], writes=[yj], ctr=k.c_dg)
                        k.V(lambda blk=blk, j=j, yj=yj, x1=x1: nc.vector.scalar_tensor_tensor(out=x1[:], in0=yj[:], scalar=rt_gate[:, blk, j:j + 1], in1=x1[:],
                                                                                op0=ALU.mult, op1=ALU.add), [yj, rt_gate, x1], [x1])
                    for fb in range(4):
                        k.V(lambda fb=fb: nc.vector.bn_stats(out=st6[:, fb, :], in_=x1[:, fb * 512:(fb + 1) * 512]), [x1], [st6])
                    k.V(lambda: nc.vector.bn_aggr(out=mv[:], in_=st6[:].rearrange("p a b -> p (a b)")), [st6], [mv])
                    k.V(lambda: nc.vector.tensor_scalar(out=rstd[:], in0=mv[:, 1:2], scalar1=EPS, scalar2=None, op0=ALU.add), [mv], [rstd])
                    k.A(lambda: nc.scalar.activation(out=rstd[:], in_=rstd[:], func=AF.Sqrt), [rstd], [rstd])
                    k.V(lambda: nc.vector.reciprocal(out=rstd[:], in_=rstd[:]), [rstd], [rstd])
                    k.V(lambda: nc.vector.tensor_scalar(out=x1[:], in0=x1[:], scalar1=mv[:, 0:1], scalar2=rstd[:, 0:1], op0=ALU.subtract, op1=ALU.mult), [x1, mv, rstd], [x1])
                    k.V(lambda: nc.vector.tensor_tensor(out=x1[:], in0=x1[:], in1=g2[:], op=ALU.mult), [x1, g2], [x1])
                    k.V(lambda: nc.vector.tensor_tensor(out=x1[:], in0=x1[:], in1=b2[:], op=ALU.add), [x1, b2], [x1])
                    k.store_sp(out, out.t.ap()[vc * TOK + blk * 128:vc * TOK + (blk + 1) * 128, :], x1, x1[:])
            k.es = es

        for vc in range(nvc):
            body(vc)
        _finish(nc, k, out)
    return nc


def _finish(nc, k, out):
    k.barrier()


def _consts():
    bf = ml_dtypes.bfloat16
    c = {}
    c["c_identb"] = np.eye(128, dtype=np.float32).astype(bf)
    c["c_identf"] = np.eye(128, dtype=np.float32)
    i = np.arange(128)[:, None]; j = np.arange(128)[None, :]
    nm = np.zeros((128, 2, 128), np.float32)
    nm[:, 0, :] = np.where(j <= i, 0.0, NEG)
    nm[:, 1, :] = np.where(i <= j, 0.0, NEG)
    c["c_negmask"] = nm.astype(bf)
    rs = np.zeros((128, 32), np.float32)
    for m in range(16):
        rs[m + 16, m] = 1.0
        rs[m, m + 16] = 1.0
    c["c_rsw"] = rs.astype(bf)
    invf = (np.float32(500000.0) ** (-np.arange(0, 32, 2, dtype=np.float32) / np.float32(32))).astype(np.float32)
    cf = np.zeros((32, 2), np.float32)
    cf[:, 0] = np.concatenate([invf, invf])
    cf[:, 1] = np.concatenate([-np.ones(16), np.ones(16)])
    c["c_invf"] = cf
    c["c_tri"] = (i < j).astype(np.float32)
    c["c_onesf"] = np.ones((128, 128), np.float32)
    c["c_onesb"] = np.ones((128, 128), np.float32).astype(bf)
    io = np.zeros((128, 2, NE), np.float32)
    io[:, 0, :] = np.arange(NE)[None, :]
    io[:, 1, :] = np.arange(NE)[None, :] * CAP
    c["c_iota"] = io
    return c


def _vbias(core):
    s = core * TOK
    vb = np.zeros((128, 69), np.float32)
    ci = 0
    for g, Dl in enumerate(DIL):
        nb = TOK // Dl // 128
        for r in range(Dl):
            for m in range(nb + 1):
                off = (HALO // Dl - 64 + 128 * m) * Dl + r
                xs = off + np.arange(128) * Dl
                glob = s - HALO + xs
                vb[:, ci] = np.where((glob >= 0) & (glob < SEQ), 0.0, NEG)
                ci += 1
    return vb


_NC_CACHE = {}
NCK = 8
NVC = NCORES // NCK


def make_in_map(x, positions, w_in, w_attn_out, sgu_ln_g, sgu_ln_b, sgu_w, sgu_b, w_sgu_out, w_out,
                ln1_g, ln1_b, w_router, b_router, w_gate_up, b_gate_up, w_down, b_down, ln2_g, ln2_b):
    f = np.float32
    x2 = np.asarray(x, f).reshape(SEQ, D)
    xpad = np.zeros((SEQ + 2 * HALO, D), f)
    xpad[HALO:HALO + SEQ] = x2
    ppad = np.zeros((1, SEQ + 2 * HALO), np.int32)
    ppad[0, HALO:HALO + SEQ] = np.asarray(positions, np.int32).reshape(SEQ)
    m = {
        "x_ext": xpad, "pos": ppad, "vbias": np.stack([_vbias(c) for c in range(NCORES)], 0),
        "w_in": np.asarray(w_in, f)[0], "w_ao": np.asarray(w_attn_out, f)[0], "w_so": np.asarray(w_sgu_out, f)[0],
        "w_o": np.asarray(w_out, f)[0],
        "w_gu": np.asarray(w_gate_up, f)[0].reshape(NE * D, 2 * DH), "w_d": np.asarray(w_down, f)[0].reshape(NE * DH, D),
        "sgu_g": np.asarray(sgu_ln_g, f).reshape(1, 1024), "sgu_bb": np.asarray(sgu_ln_b, f).reshape(1, 1024),
        "sgu_wT": np.ascontiguousarray(np.asarray(sgu_w, f)[0].transpose(2, 0, 1)),
        "sgu_bias": np.asarray(sgu_b, f).reshape(1, 1024),
        "ln1g": np.asarray(ln1_g, f).reshape(1, D), "ln1b": np.asarray(ln1_b, f).reshape(1, D),
        "ln2g": np.asarray(ln2_g, f).reshape(1, D), "ln2b": np.asarray(ln2_b, f).reshape(1, D),
        "w_r": np.ascontiguousarray(np.asarray(w_router, f)[0].reshape(KC, 128, NE).transpose(1, 0, 2)),
        "b_r": np.asarray(b_router, f).reshape(1, NE),
        "b_gu": np.ascontiguousarray(np.asarray(b_gate_up, f)[0].reshape(NE, 32, 128).transpose(2, 0, 1)),
        "b_d": np.asarray(b_down, f)[0],
        **_consts(),
    }
    return m


def kernel(**inputs):
    m = make_in_map(**inputs)
    span = NVC * TOK
    maps = []
    for c in range(NCK):
        mc = dict(m)
        mc["x_ext"] = m["x_ext"][c * span:c * span + span + 2 * HALO]
        mc["pos"] = np.ascontiguousarray(m["pos"][:, c * span:c * span + span + 2 * HALO])
        mc["vbias"] = np.ascontiguousarray(m["vbias"][c * NVC:(c + 1) * NVC])
        maps.append(mc)
    if "nc" not in _NC_CACHE:
        _NC_CACHE["nc"] = build_nc(nvc=NVC)
    res = run_bass_kernel_spmd(_NC_CACHE["nc"], maps, core_ids=list(range(NCK)))
    outp = np.concatenate([np.asarray(r["out"], np.float32) for r in res.results], axis=0)
    return outp.reshape(1, SEQ, D)
```
